# Optimizing a Trainium2 kernel written in Bass

```python
import math
import jax, jax.numpy as jnp
from jax import lax
import numpy as np

D_MODEL = 1024
BATCH = 8
SEQ = 4096
DEPTH = 2

HEAD_DIM = 64
NSA_HEADS = 8
NSA_GROUPS = 2
NSA_HPG = NSA_HEADS // NSA_GROUPS
CMP_BLOCK = 32
CMP_STRIDE = 16
CMP_HIDDEN = 256
SEL_BLOCK = 64
N_SEL = 8
WINDOW = 512
FORCE_BONUS = 1e4
DSA_HEADS = 8
IDX_HEADS = 4
IDX_DIM = 64
DSA_TOPK_MAX = 256
Q_BLOCK = 128
N_BUCKETS = 32
T5_MAX_DIST = 128
N_ATTN_HEADS = NSA_HEADS + DSA_HEADS
LRU_WIDTH = 512
LRU_BLOCKS = 8
LRU_BLOCK_DIM = LRU_WIDTH // LRU_BLOCKS
CONV_WIDTH = 4
LRU_C = 8.0
MLSTM_HEADS = 4
MLSTM_DIM = 128
MLSTM_WIDTH = MLSTM_HEADS * MLSTM_DIM
MLSTM_CHUNK = 64
PLE_DIM = 256
RMS_EPS = 1e-6
NEG = -1e30
N_EVEN = (DEPTH + 1) // 2
N_ODD = DEPTH // 2
NSA_WIDTH = NSA_HEADS * HEAD_DIM
DSA_WIDTH = DSA_HEADS * HEAD_DIM
NSA_KV = NSA_GROUPS * HEAD_DIM
ATTN_SPLITS = (NSA_WIDTH, 6 * NSA_KV, 3 * NSA_HEADS, NSA_WIDTH,
               DSA_WIDTH, HEAD_DIM, HEAD_DIM, IDX_HEADS * IDX_DIM, IDX_DIM, IDX_HEADS, DSA_WIDTH)
ATTN_IN = sum(ATTN_SPLITS)
ATTN_OUT = NSA_WIDTH + DSA_WIDTH
REC_SPLITS = (LRU_WIDTH, LRU_WIDTH, MLSTM_WIDTH, MLSTM_WIDTH, MLSTM_WIDTH,
              MLSTM_HEADS, MLSTM_HEADS, MLSTM_WIDTH, MLSTM_WIDTH)
REC_IN = sum(REC_SPLITS)
REC_OUT = LRU_WIDTH + MLSTM_WIDTH

kernel_name = "hybrid_nsa_dsa_rglru_mlstm_trunk"


def rmsnorm(x, g):
    xf = x.astype(jnp.float32)
    y = xf * lax.rsqrt(jnp.mean(xf * xf, axis=-1, keepdims=True) + RMS_EPS)
    return (y * g.astype(jnp.float32)).astype(x.dtype)


def split_cols(u, widths):
    return jnp.split(u, [int(c) for c in np.cumsum(widths)[:-1]], axis=-1)


def masked_softmax(s, mask):
    return jax.nn.softmax(jnp.where(mask, s, NEG), axis=-1)


def t5_bucket(dist):
    n = jnp.maximum(dist, 0)
    max_exact = N_BUCKETS // 2
    nf = jnp.maximum(n, 1).astype(jnp.float32)
    large = max_exact + (jnp.log(nf / max_exact) / math.log(T5_MAX_DIST / max_exact)
                         * (N_BUCKETS - max_exact)).astype(jnp.int32)
    large = jnp.minimum(large, N_BUCKETS - 1)
    return jnp.where(n < max_exact, n, large)


def causal_dwconv(x, w, b):
    y = lax.conv_general_dilated(x, w[:, None, :], window_strides=(1,),
                                 padding=[(CONV_WIDTH - 1, 0)],
                                 dimension_numbers=('NWC', 'WIO', 'NWC'),
                                 feature_group_count=x.shape[-1])
    return y + b


def compress_blocks(k, pos, w1, w2):
    B, S = k.shape[0], k.shape[1]
    n_cmp = (S - CMP_BLOCK) // CMP_STRIDE + 1
    idx = np.arange(n_cmp)[:, None] * CMP_STRIDE + np.arange(CMP_BLOCK)[None, :]
    blk = k[:, idx] + pos[None, None, :, None, :]
    blk = blk.transpose(0, 1, 3, 2, 4).reshape(B, n_cmp, NSA_GROUPS, CMP_BLOCK * HEAD_DIM)
    return jax.nn.silu(blk @ w1) @ w2


def sparse_attention_layer(h, w_in, w_out, cmp_pos_k, cmp_w1_k, cmp_w2_k,
                           cmp_pos_v, cmp_w1_v, cmp_w2_v, t5_table):
    f32 = jnp.float32
    B, S, _ = h.shape
    u = h @ w_in
    (a_q, a_kv, a_g, a_z, b_q, b_k, b_v, b_qi, b_ki, b_wi, b_z) = split_cols(u, ATTN_SPLITS)
    scale = HEAD_DIM ** -0.5

    a_q = a_q.astype(f32).reshape(B, S, NSA_GROUPS, NSA_HPG, HEAD_DIM)
    gates = jax.nn.sigmoid(a_g.astype(f32)).reshape(B, S, NSA_GROUPS, NSA_HPG, 3)
    kc, vc, ks, vs, kw, vw = [t.reshape(B, S, NSA_GROUPS, HEAD_DIM) for t in jnp.split(a_kv, 6, axis=-1)]
    k_cmp = compress_blocks(kc, cmp_pos_k, cmp_w1_k, cmp_w2_k).astype(f32)
    v_cmp = compress_blocks(vc, cmp_pos_v, cmp_w1_v, cmp_w2_v).astype(f32)
    n_cmp = k_cmp.shape[1]
    cmp_end = jnp.asarray(np.arange(n_cmp) * CMP_STRIDE + CMP_BLOCK - 1, jnp.int32)
    n_blocks = S // SEL_BLOCK
    n_pick = min(N_SEL, n_blocks)
    ci = np.arange(n_cmp)[:, None]
    sj = np.arange(n_blocks)[None, :]
    overlap = jnp.asarray(((ci * CMP_STRIDE < (sj + 1) * SEL_BLOCK)
                           & (ci * CMP_STRIDE + CMP_BLOCK > sj * SEL_BLOCK)).astype(np.float32))
    blk_start = jnp.arange(n_blocks, dtype=jnp.int32) * SEL_BLOCK
    nb_idx = jnp.arange(n_blocks, dtype=jnp.int32)
    ks_blocks = ks.astype(f32).transpose(0, 2, 1, 3).reshape(B, NSA_GROUPS, n_blocks, SEL_BLOCK, HEAD_DIM)
    vs_blocks = vs.astype(f32).transpose(0, 2, 1, 3).reshape(B, NSA_GROUPS, n_blocks, SEL_BLOCK, HEAD_DIM)
    pad = ((0, 0), (WINDOW, 0), (0, 0), (0, 0))
    kw_pad = jnp.pad(kw.astype(f32), pad)
    vw_pad = jnp.pad(vw.astype(f32), pad)
    tbl_a = t5_table[:, :NSA_HEADS].astype(f32).reshape(N_BUCKETS, NSA_GROUPS, NSA_HPG).transpose(1, 2,0)
    g_ar = jnp.arange(NSA_GROUPS)[None, :, None, None, None]
    h_ar = jnp.arange(NSA_HPG)[None, None, :, None, None]

    b_q = b_q.astype(f32).reshape(B, S, DSA_HEADS, HEAD_DIM)
    b_k = b_k.astype(f32)
    b_v = b_v.astype(f32)
    b_qi = b_qi.astype(f32).reshape(B, S, IDX_HEADS, IDX_DIM)
    b_ki = b_ki.astype(f32)
    b_wi = b_wi.astype(f32) * (IDX_DIM ** -0.5 * IDX_HEADS ** -0.5)
    k_top = min(DSA_TOPK_MAX, S // 4)
    tbl_b = t5_table[:, NSA_HEADS:].astype(f32).T
    key_pos = jnp.arange(S, dtype=jnp.int32)

    def block_fn(qb):
        q0 = qb * Q_BLOCK
        t = q0 + jnp.arange(Q_BLOCK, dtype=jnp.int32)
        qa = lax.dynamic_slice_in_dim(a_q, q0, Q_BLOCK, axis=1)
        dist_c = t[:, None] - cmp_end[None, :]
        mask_c = dist_c >= 0
        s_c = jnp.einsum('bqghd,bcgd->bghqc', qa, k_cmp) * scale + tbl_a[:, :, t5_bucket(dist_c)]
        p_c = masked_softmax(s_c, mask_c) * jnp.any(mask_c, axis=-1)[:, None].astype(f32)
        o_c = jnp.einsum('bghqc,bcgd->bqghd', p_c, v_cmp)
        imp = jnp.einsum('bghqc,cn->bgqn', p_c, overlap)
        cur = t // SEL_BLOCK
        forced = ((nb_idx[None, :] == 0) | (nb_idx[None, :] == cur[:, None])
                  | (nb_idx[None, :] == cur[:, None] - 1)).astype(f32)
        admissible = blk_start[None, :] <= t[:, None]
        score = jnp.where(admissible, imp + FORCE_BONUS * forced, NEG)
        _, sel = lax.top_k(score, n_pick)
        gather = jax.vmap(jax.vmap(lambda kb, ix: kb[ix]))
        k_sel = gather(ks_blocks, sel).reshape(B, NSA_GROUPS, Q_BLOCK, n_pick * SEL_BLOCK, HEAD_DIM)
        v_sel = gather(vs_blocks, sel).reshape(B, NSA_GROUPS, Q_BLOCK, n_pick * SEL_BLOCK, HEAD_DIM)
        pos_sel = (sel[..., None] * SEL_BLOCK + jnp.arange(SEL_BLOCK, dtype=jnp.int32)).reshape(
            B, NSA_GROUPS, Q_BLOCK, n_pick * SEL_BLOCK)
        dist_s = t[None, None, :, None] - pos_sel
        bias_s = tbl_a[g_ar, h_ar, t5_bucket(dist_s)[:, :, None]]
        s_s = jnp.einsum('bqghd,bgqkd->bghqk', qa, k_sel) * scale + bias_s
        p_s = masked_softmax(s_s, (dist_s >= 0)[:, :, None])
        o_s = jnp.einsum('bghqk,bgqkd->bqghd', p_s, v_sel)
        k_win = lax.dynamic_slice_in_dim(kw_pad, q0, Q_BLOCK + WINDOW, axis=1)
        v_win = lax.dynamic_slice_in_dim(vw_pad, q0, Q_BLOCK + WINDOW, axis=1)
        pos_w = q0 - WINDOW + jnp.arange(Q_BLOCK + WINDOW, dtype=jnp.int32)
        dist_w = t[:, None] - pos_w[None, :]
        mask_w = (dist_w >= 0) & (dist_w < WINDOW) & (pos_w >= 0)[None, :]
        s_w = jnp.einsum('bqghd,bkgd->bghqk', qa, k_win) * scale + tbl_a[:, :, t5_bucket(dist_w)]
        p_w = masked_softmax(s_w, mask_w)
        o_w = jnp.einsum('bghqk,bkgd->bqghd', p_w, v_win)
        g = lax.dynamic_slice_in_dim(gates, q0, Q_BLOCK, axis=1)
        o_a = (g[..., 0:1] * o_c + g[..., 1:2] * o_s + g[..., 2:3] * o_w).reshape(B, Q_BLOCK, NSA_WIDTH)
        qi = lax.dynamic_slice_in_dim(b_qi, q0, Q_BLOCK, axis=1)
        wi = lax.dynamic_slice_in_dim(b_wi, q0, Q_BLOCK, axis=1)
        idx_score = jnp.einsum('bqh,bqhs->bqs', wi, jax.nn.relu(jnp.einsum('bqhd,bsd->bqhs', qi, b_ki)))
        idx_score = jnp.where(key_pos[None, :] <= t[:, None], idx_score, NEG)
        _, sel_b = lax.top_k(idx_score, k_top)
        take = jax.vmap(lambda kk, ix: kk[ix])
        k_b = take(b_k, sel_b)
        v_b = take(b_v, sel_b)
        dist_b = t[None, :, None] - sel_b
        bias_b = tbl_b[:, t5_bucket(dist_b)].transpose(1, 0, 2, 3)
        qd = lax.dynamic_slice_in_dim(b_q, q0, Q_BLOCK, axis=1)
        s_b = jnp.einsum('bqhd,bqkd->bhqk', qd, k_b) * scale + bias_b
        p_b = masked_softmax(s_b, (dist_b >= 0)[:, None])
        o_b = jnp.einsum('bhqk,bqkd->bqhd', p_b, v_b).reshape(B, Q_BLOCK, DSA_WIDTH)
        return o_a, o_b

    o_a, o_b = lax.map(block_fn, jnp.arange(S // Q_BLOCK, dtype=jnp.int32))
    o_a = o_a.transpose(1, 0, 2, 3).reshape(B, S, NSA_WIDTH)
    o_b = o_b.transpose(1, 0, 2, 3).reshape(B, S, DSA_WIDTH)
    y = jnp.concatenate([o_a * jax.nn.silu(a_z.astype(f32)), o_b * jax.nn.silu(b_z.astype(f32))], axis=-1)
    return y.astype(h.dtype) @ w_out


def rglru(x, conv_w, conv_b, wa, ba, wx, bx, lam):
    B, S, _ = x.shape
    xc = causal_dwconv(x, conv_w, conv_b)
    xb = xc.reshape(B, S, LRU_BLOCKS, LRU_BLOCK_DIM)
    r = jax.nn.sigmoid(jnp.einsum('bsgi,gij->bsgj', xb, wa).reshape(B, S, LRU_WIDTH) + ba)
    ig = jax.nn.sigmoid(jnp.einsum('bsgi,gij->bsgj', xb, wx).reshape(B, S, LRU_WIDTH) + bx)
    log_a = -LRU_C * r.astype(jnp.float32) * jax.nn.softplus(-lam.astype(jnp.float32))
    a = jnp.exp(log_a)
    b = jnp.sqrt(-jnp.expm1(2.0 * log_a)) * (ig * xc).astype(jnp.float32)

    def combine(lhs, rhs):
        a1, b1 = lhs
        a2, b2 = rhs
        return a1 * a2, a2 * b1 + b2

    _, hs = lax.associative_scan(combine, (a, b), axis=1)
    return hs


def mlstm_chunkwise(q, k, v, i_pre, f_pre):
    f32 = jnp.float32
    B, S, _ = q.shape
    L = MLSTM_CHUNK
    nc = S // L

    def to_chunks(t):
        return t.astype(f32).reshape(B, nc, L, MLSTM_HEADS, MLSTM_DIM).transpose(1, 0, 3, 2, 4)

    def gate_chunks(t):
        return t.reshape(B, nc, L, MLSTM_HEADS).transpose(1, 0, 3, 2)

    qc = to_chunks(q)
    kc = to_chunks(k) * (MLSTM_DIM ** -0.5)
    vc = to_chunks(v)
    li = gate_chunks(i_pre.astype(f32))
    lf = gate_chunks(jax.nn.log_sigmoid(f_pre.astype(f32)))
    causal = jnp.asarray(np.tril(np.ones((L, L), dtype=bool)))

    def step(carry, xs):
        C, n, m = carry
        q_, k_, v_, li_, lf_ = xs
        b = jnp.cumsum(lf_, axis=-1)
        dmat = jnp.where(causal, b[..., :, None] - b[..., None, :] + li_[..., None, :], -jnp.inf)
        inter = b + m[..., None]
        m_t = jnp.maximum(inter, jnp.max(dmat, axis=-1))
        w = jnp.einsum('bhld,bhsd->bhls', q_, k_) * jnp.exp(dmat - m_t[..., None])
        prev = jnp.exp(inter - m_t)
        num = prev[..., None] * jnp.einsum('bhld,bhde->bhle', q_, C) + jnp.einsum('bhls,bhse->bhle', w, v_)
        den = prev * jnp.einsum('bhld,bhd->bhl', q_, n) + jnp.sum(w, axis=-1)
        out = num / jnp.maximum(jnp.abs(den), jnp.exp(-m_t))[..., None]
        b_last = b[..., -1]
        decay = b_last[..., None] - b + li_
        m_new = jnp.maximum(b_last + m, jnp.max(decay, axis=-1))
        wk = jnp.exp(decay - m_new[..., None])
        keep = jnp.exp(b_last + m - m_new)
        C_new = keep[..., None, None] * C + jnp.einsum('bhs,bhsd,bhse->bhde', wk, k_, v_)
        n_new = keep[..., None] * n + jnp.einsum('bhs,bhsd->bhd', wk, k_)
        return (C_new, n_new, m_new), out

    init = (jnp.zeros((B, MLSTM_HEADS, MLSTM_DIM, MLSTM_DIM), f32),
            jnp.zeros((B, MLSTM_HEADS, MLSTM_DIM), f32),
            jnp.zeros((B, MLSTM_HEADS), f32))
    _, hs = lax.scan(step, init, (qc, kc, vc, li, lf))
    return hs.transpose(1, 0, 3, 2, 4).reshape(B, S, MLSTM_WIDTH)


def recurrent_layer(h, w_in, w_out, conv_c_w, conv_c_b, wa, ba, wx, bx, lam,
                    conv_d_w, conv_d_b, b_i, b_f):
    f32 = jnp.float32
    u = h @ w_in
    c_x, c_z, d_q, d_k, d_v, d_i, d_f, d_o, d_z = split_cols(u, REC_SPLITS)
    y_c = rglru(c_x, conv_c_w, conv_c_b, wa, ba, wx, bx, lam) * jax.nn.silu(c_z.astype(f32))
    qk = jax.nn.silu(causal_dwconv(jnp.concatenate([d_q, d_k], axis=-1), conv_d_w, conv_d_b))
    q, k = jnp.split(qk, 2, axis=-1)
    h_d = mlstm_chunkwise(q, k, d_v, d_i + b_i, d_f + b_f)
    y_d = jax.nn.sigmoid(d_o.astype(f32)) * h_d * jax.nn.silu(d_z.astype(f32))
    y = jnp.concatenate([y_c, y_d], axis=-1)
    return y.astype(h.dtype) @ w_out


def setup_inputs(seed: int = 0) -> dict:
    key = jax.random.key(seed)
    keys = iter(jax.random.split(key, 40))

    def nrm(shape, scale):
        return jax.random.normal(next(keys), shape, jnp.float32) * scale

    a0 = jax.random.uniform(next(keys), (N_ODD, LRU_WIDTH), jnp.float32, minval=0.9, maxval=0.999)
    return {
        "x": nrm((BATCH, SEQ, D_MODEL), 1.0),
        "p": nrm((DEPTH, BATCH, SEQ, PLE_DIM), 1.0),
        "norm_g": 1.0 + nrm((DEPTH, D_MODEL), 0.02),
        "final_g": 1.0 + nrm((D_MODEL,), 0.02),
        "ple_w": nrm((DEPTH, PLE_DIM, D_MODEL), PLE_DIM ** -0.5),
        "ple_gate_w": nrm((DEPTH, D_MODEL, D_MODEL), D_MODEL ** -0.5),
        "t5_table": nrm((N_BUCKETS, N_ATTN_HEADS), 0.3),
        "attn_w_in": nrm((N_EVEN, D_MODEL, ATTN_IN), D_MODEL ** -0.5),
        "attn_w_out": nrm((N_EVEN, ATTN_OUT, D_MODEL), ATTN_OUT ** -0.5),
        "cmp_pos_k": nrm((N_EVEN, CMP_BLOCK, HEAD_DIM), 0.1),
        "cmp_w1_k": nrm((N_EVEN, CMP_BLOCK * HEAD_DIM, CMP_HIDDEN), (CMP_BLOCK * HEAD_DIM) ** -0.5),
        "cmp_w2_k": nrm((N_EVEN, CMP_HIDDEN, HEAD_DIM), CMP_HIDDEN ** -0.5),
        "cmp_pos_v": nrm((N_EVEN, CMP_BLOCK, HEAD_DIM), 0.1),
        "cmp_w1_v": nrm((N_EVEN, CMP_BLOCK * HEAD_DIM, CMP_HIDDEN), (CMP_BLOCK * HEAD_DIM) ** -0.5),
        "cmp_w2_v": nrm((N_EVEN, CMP_HIDDEN, HEAD_DIM), CMP_HIDDEN ** -0.5),
        "rec_w_in": nrm((N_ODD, D_MODEL, REC_IN), D_MODEL ** -0.5),
        "rec_w_out": nrm((N_ODD, REC_OUT, D_MODEL), REC_OUT ** -0.5),
        "lru_conv_w": nrm((N_ODD, CONV_WIDTH, LRU_WIDTH), CONV_WIDTH ** -0.5),
        "lru_conv_b": nrm((N_ODD, LRU_WIDTH), 0.01),
        "lru_wa": nrm((N_ODD, LRU_BLOCKS, LRU_BLOCK_DIM, LRU_BLOCK_DIM), LRU_BLOCK_DIM ** -0.5),
        "lru_ba": nrm((N_ODD, LRU_WIDTH), 0.01),
        "lru_wx": nrm((N_ODD, LRU_BLOCKS, LRU_BLOCK_DIM, LRU_BLOCK_DIM), LRU_BLOCK_DIM ** -0.5),
        "lru_bx": nrm((N_ODD, LRU_WIDTH), 0.01),
        "lru_lambda": jnp.log(a0) - jnp.log1p(-a0),
        "mlstm_conv_w": nrm((N_ODD, CONV_WIDTH, 2 * MLSTM_WIDTH), CONV_WIDTH ** -0.5),
        "mlstm_conv_b": nrm((N_ODD, 2 * MLSTM_WIDTH), 0.01),
        "mlstm_b_i": nrm((N_ODD, MLSTM_HEADS), 0.1),
        "mlstm_b_f": jnp.linspace(3.0, 6.0, MLSTM_HEADS, dtype=jnp.float32)[None, :] + nrm((N_ODD, MLSTM_HEADS), 0.1),
    }


def reference(x, p, norm_g, final_g, ple_w, ple_gate_w, t5_table,
              attn_w_in, attn_w_out, cmp_pos_k, cmp_w1_k, cmp_w2_k,
              cmp_pos_v, cmp_w1_v, cmp_w2_v,
              rec_w_in, rec_w_out, lru_conv_w, lru_conv_b, lru_wa, lru_ba,
              lru_wx, lru_bx, lru_lambda, mlstm_conv_w, mlstm_conv_b, mlstm_b_i, mlstm_b_f):
    for i in range(DEPTH):
        hn = rmsnorm(x, norm_g[i])
        j = i // 2
        if i % 2 == 0:
            y = sparse_attention_layer(hn, attn_w_in[j], attn_w_out[j],
                                       cmp_pos_k[j], cmp_w1_k[j], cmp_w2_k[j],
                                       cmp_pos_v[j], cmp_w1_v[j], cmp_w2_v[j], t5_table)
        else:
            y = recurrent_layer(hn, rec_w_in[j], rec_w_out[j], lru_conv_w[j], lru_conv_b[j],
                                lru_wa[j], lru_ba[j], lru_wx[j], lru_bx[j], lru_lambda[j],
                                mlstm_conv_w[j], mlstm_conv_b[j], mlstm_b_i[j], mlstm_b_f[j])
        x = x + y
        x = x + (p[i] @ ple_w[i]) * jax.nn.sigmoid(x @ ple_gate_w[i])
    return rmsnorm(x, final_g)
```

```python
import math
from contextlib import ExitStack

import numpy as np
import ml_dtypes

import concourse.bass as bass
import concourse.mybir as mybir
from concourse.bass_utils import run_bass_kernel_spmd

F32 = mybir.dt.float32
BF16 = mybir.dt.bfloat16
I32 = mybir.dt.int32
AF = mybir.ActivationFunctionType
ALU = mybir.AluOpType
AX = mybir.AxisListType

D = 1024
NEGM = -30000.0


class Buf:
    __slots__ = ("w", "r", "name")

    def __init__(self, name=""):
        self.w = None
        self.r = {}
        self.name = name


def bufs(n, name=""):
    return [Buf(f"{name}{i}") for i in range(n)]


class Sched:
    ENG = ("pe", "act", "dve", "pool", "sp")
    NDMA = 16

    def __init__(self, nc):
        self.nc = nc
        self.eobj = {"pe": nc.tensor, "act": nc.scalar, "dve": nc.vector, "pool": nc.gpsimd, "sp": nc.sync}
        self.sem = {}
        for e in self.ENG:
            self.sem[e] = nc.alloc_semaphore(f"s_{e}")
        self.cnt = {e: 0 for e in self.ENG}
        self.seen = {e: {} for e in self.ENG}
        self.snap = {}
        self.prog = {e: [] for e in self.ENG}
        self.dma_i = {}
        self.dma_val = {}
        for q in ("sp", "pool", "act"):
            for k in range(self.NDMA):
                key = f"d_{q}{k}"
                self.sem[key] = nc.alloc_semaphore(key)
                self.dma_val[key] = 0
            self.dma_i[q] = 0
        self.n_ops = 0
        self.n_waits = 0
        self.limit = None
        self.pending_fence = {e: [] for e in self.ENG}

    def fence(self):
        toks = [(e, self.cnt[e]) for e in self.ENG if self.cnt[e] > 0]
        toks += [(k, v) for k, v in self.dma_val.items() if v > 0]
        for e in self.ENG:
            self.pending_fence[e] = list(toks)

    def _collect(self, eng, reads, writes, extra=()):
        need = {}

        def add(tok):
            if tok is None:
                return
            k, v = tok
            if k == eng and eng == "pe":
                return
            if self.seen[eng].get(k, 0) >= v:
                return
            if need.get(k, 0) < v:
                need[k] = v

        for b in reads:
            add(b.w)
        for b in writes:
            add(b.w)
            for k, v in b.r.items():
                add((k, v))
        for t in extra:
            add(t)
        if self.pending_fence[eng]:
            for t in self.pending_fence[eng]:
                if not (t[0] == eng and eng in ("pe",)) or True:
                    k, v = t
                    if self.seen[eng].get(k, 0) < v and need.get(k, 0) < v and not (k == eng and False):
                        need[k] = v
            self.pending_fence[eng] = []
        sn = self.seen[eng]
        for k, v in need.items():
            sn[k] = max(sn.get(k, 0), v)
            other = self.snap.get((k, v))
            if other:
                for k2, v2 in other.items():
                    if sn.get(k2, 0) < v2:
                        sn[k2] = v2
        return list(need.items())

    def op(self, eng, meth, reads, writes, *a, **kw):
        if self.limit is not None and self.n_ops >= self.limit:
            return None
        waits = self._collect(eng, reads, writes)
        self.cnt[eng] += 1
        me = (eng, self.cnt[eng])
        for b in reads:
            b.r[eng] = me[1]
        for b in writes:
            b.w = me
            b.r = {}
        self.snap[me] = dict(self.seen[eng])
        self.prog[eng].append((waits, (meth, a, kw), self.sem[eng], 1))
        self.n_ops += 1
        self.n_waits += len(waits)
        return me

    def pe(self, reads, writes, meth, *a, **kw):
        return self.op("pe", meth, reads, writes, *a, **kw)

    def act(self, reads, writes, meth, *a, **kw):
        return self.op("act", meth, reads, writes, *a, **kw)

    def dve(self, reads, writes, meth, *a, **kw):
        return self.op("dve", meth, reads, writes, *a, **kw)

    def pool(self, reads, writes, meth, *a, **kw):
        return self.op("pool", meth, reads, writes, *a, **kw)

    def dma(self, q, out, in_, reads=(), writes=(), **kw):
        if self.limit is not None and self.n_ops >= self.limit:
            return None
        i = self.dma_i[q]
        self.dma_i[q] += 1
        key = f"d_{q}{i % self.NDMA}"
        prev = self.dma_val[key]
        extra = [(key, prev)] if prev > 0 else []
        waits = self._collect(q, reads, writes, extra)
        self.dma_val[key] = prev + 16
        me = (key, prev + 16)
        for b in reads:
            b.r[key] = me[1]
        for b in writes:
            b.w = me
            b.r = {}
        kw = dict(kw, out=out, in_=in_)
        self.prog[q].append((waits, ("dma_start", (), kw), self.sem[key], 16))
        self.n_ops += 1
        self.n_waits += len(waits)
        return me

    def wait_all(self, eng, toks):
        waits = []
        for t in toks:
            if t is not None:
                waits.append(t)
        if self.limit is not None:
            waits = [(e, self.cnt[e]) for e in self.ENG if self.cnt[e] > 0]
            waits += [(k, v) for k, v in self.dma_val.items() if v > 0]
        self.prog[eng].append((waits, None, None, 0))

    def emit(self):
        nc = self.nc
        with nc.Block() as block:
            def run(engname):
                def body(e):
                    regcache = {}
                    for waits, fn, sem, inc in self.prog[engname]:
                        for k, v in waits:
                            e.wait_ge(self.sem[k], v)
                        if fn is not None:
                            meth, a, kw = fn
                            if meth == "affine_select":
                                fv = kw["fill"]
                                if fv not in regcache:
                                    regcache[fv] = e.to_reg(fv)
                                kw = dict(kw, fill=regcache[fv])
                            try:
                                ins = getattr(e, meth)(*a, **kw)
                            except Exception:
                                print("EMIT FAIL", engname, meth, [getattr(x, "shape", x) for x in a],
                                      {k: (getattr(v, "shape", v), getattr(v, "ap", None)) for k, v in kw.items()})
                                raise
                            ins.then_inc(sem, inc)
                return body
            block.tensor(run("pe"))
            block.scalar(run("act"))
            block.vector(run("dve"))
            block.gpsimd(run("pool"))
            block.sync(run("sp"))


ATTN_IN = 3292
REC_IN = 3592
RMS_EPS = 1e-6

FM0 = [("qa", 512), ("qb", 512), ("qi", 256), ("kc", 128), ("vc", 128), ("ks", 128), ("kw", 128),
       ("kbki", 128), ("za", 512), ("zb", 512), ("g", 24)]
FM0_COLS = sum(c for _, c in FM0)
TM0_COLS = 324


def _perm_attn_w_in(w):
    o = {}
    o["a_q"] = 0
    o["a_kv"] = 512
    o["a_g"] = 1280
    o["a_z"] = 1304
    o["b_q"] = 1816
    o["b_k"] = 2328
    o["b_v"] = 2392
    o["b_qi"] = 2456
    o["b_ki"] = 2712
    o["b_wi"] = 2776
    o["b_z"] = 2780
    qa = []
    for m in range(4):
        for g in range(2):
            st = o["a_q"] + g * 256 + m * 64
            qa.append(np.arange(st, st + 64))
    qa = np.concatenate(qa)
    kv = o["a_kv"]
    cols_fm = np.concatenate([
        qa,
        np.arange(o["b_q"], o["b_q"] + 512),
        np.arange(o["b_qi"], o["b_qi"] + 256),
        np.arange(kv + 0, kv + 128),
        np.arange(kv + 128, kv + 256),
        np.arange(kv + 256, kv + 384),
        np.arange(kv + 512, kv + 640),
        np.arange(o["b_k"], o["b_k"] + 64), np.arange(o["b_ki"], o["b_ki"] + 64),
        np.arange(o["a_z"], o["a_z"] + 512),
        np.arange(o["b_z"], o["b_z"] + 512),
        np.arange(o["a_g"], o["a_g"] + 24),
    ])
    cols_tm = np.concatenate([
        np.arange(kv + 384, kv + 512),
        np.arange(kv + 640, kv + 768),
        np.arange(o["b_v"], o["b_v"] + 64),
        np.arange(o["b_wi"], o["b_wi"] + 4),
    ])
    assert len(cols_fm) == FM0_COLS and len(cols_tm) == TM0_COLS
    return np.ascontiguousarray(w[:, cols_fm]), np.ascontiguousarray(w[:, cols_tm])


class Ctx:
    pass


def bank(c, i, n=1, dt=F32):
    ap = c.psum[:, i * 512:(i + n) * 512]
    if dt != F32:
        ap = ap.bitcast(dt)
    return ap


def build_program(S, stages=("l0proj",), dbg=(), limit=None):
    nc = bass.Bass("TRN2", target_bir_lowering=False)
    NT = S // 128
    NG = S // 512
    sc = Sched(nc)
    sc.limit = limit
    es = ExitStack()
    c = Ctx()
    c.nc, c.sc, c.S, c.NT, c.NG = nc, sc, S, NT, NG
    c.dbg = dbg
    c.dbg_toks = []

    def din(name, shape, dt=F32):
        return nc.dram_tensor(name, list(shape), dt, kind="ExternalInput").ap()

    def dscr(name, shape, dt=F32):
        if name in dbg:
            return nc.dram_tensor(name, list(shape), dt, kind="ExternalOutput").ap()
        return nc.dram_tensor(name, list(shape), dt).ap()

    def dout(name, shape, dt=F32):
        return nc.dram_tensor(name, list(shape), dt, kind="ExternalOutput").ap()

    def sb(name, shape, dt=F32):
        return es.enter_context(nc.sbuf_tensor(name, list(shape), dt))

    def dump(name, ap, reads):
        if name not in dbg or sc.limit is not None:
            return
        o = dout("dbg_" + name, list(ap.shape), ap.dtype)
        c.dbg_toks.append(sc.dma("sp", o, ap, reads=reads))

    c.din, c.dscr, c.dout, c.sb, c.dump = din, dscr, dout, sb, dump

    c.x = din("x", [S, D])
    c.norm_g0 = din("norm_g0", [1, D])
    c.w0_fm = din("w0_fm", [D, FM0_COLS])
    c.w0_tm = din("w0_tm", [D, TM0_COLS])

    c.psum = es.enter_context(nc.psum_tensor("ps", [128, 4096], F32))
    c.pb = bufs(8, "psb")

    c.ident_f = sb("ident_f", [128, 128], F32)
    c.ident_b = sb("ident_b", [128, 128], BF16)
    c.eps_t = sb("eps_t", [128, 1], F32)
    c.b_const = Buf("const")
    idn = din("ident", [128, 128])
    sc.dma("sp", c.ident_f[:], idn, writes=[c.b_const])
    sc.dve([c.b_const], [c.b_const], "tensor_copy", out=c.ident_b[:], in_=c.ident_f[:])
    sc.dve([], [c.b_const], "memset", c.eps_t[:], RMS_EPS)

    finals = []
    c.hc = {}
    x1_d = dscr("x1_d", [S, D])
    if "l0" in stages:
        NB = S // 64
        for name, shape in (("Fc", [S, NB]), ("Bc", [128, 8, 16]), ("AB0", [128, 8, 128]), ("AB1", [128, 8, 128]),
                            ("DB0", [128, 8, 128]), ("DB1", [128, 8, 128]), ("W4", [128, 4, 128]), ("tb31", [1, 16]),
                            ("I4", [128, 512])):
            c.hc[name] = din("hc_" + name, shape)
        es0 = ExitStack()
        c.es = es0
        layer0_alloc(c)
        esA = ExitStack()
        c.KcT = esA.enter_context(nc.sbuf_tensor("KcT", [128, S + 32], BF16))
        c.VcT = esA.enter_context(nc.sbuf_tensor("VcT", [128, S + 32], BF16))
        finals += layer0_proj(c)
        sc.fence()
        layer0_cmp(c)
        esA.close()
        sc.fence()
        finals += layer0_attn(c, x1_d)
        es0.close()
        sc.fence()

    if "l1" in stages:
        out_d = dout("out", [S, D])
        finals += layer1(c, x1_d, out_d)

    sc.wait_all("sp", finals + c.dbg_toks)
    sc.emit()
    es.close()
    return nc


def layer0_alloc(c):
    S, NT, NG = c.S, c.NT, c.NG
    sb = lambda name, shape, dt=F32: c.es.enter_context(c.nc.sbuf_tensor(name, list(shape), dt))
    c.KsT = sb("KsT", [128, S], BF16)
    c.KwT = sb("KwT", [128, S], BF16)
    c.KbKi = sb("KbKi", [128, S], BF16)
    c.Vs = sb("Vs", [128, NT, 2, 65], BF16)
    c.Vw = sb("Vw", [128, NT, 2, 65], BF16)
    c.Vb = sb("Vb", [128, NT, 65], BF16)
    c.WI = sb("WI", [128, NT, 4], F32)
    n_cmp = S // 16 - 1
    c.KcmpT = sb("KcmpT", [128, S // 16], BF16)
    c.Vcmp = sb("Vcmp", [128, (n_cmp + 127) // 128, 2, 64], BF16)
    c.b_KcT, c.b_VcT, c.b_KsT, c.b_KwT, c.b_KbKi = (bufs(NG, n) for n in ("KcT", "VcT", "KsT", "KwT", "KbKi"))
    c.b_V = bufs(NT, "V")


def layer0_proj(c):
    nc, sc, S, NT, NG = c.nc, c.sc, c.S, c.NT, c.NG
    sb, dscr = c.sb, c.dscr
    toks = []
    c.QaT_d = dscr("QaT_d", [512, S], BF16)
    c.QbT_d = dscr("QbT_d", [512, S], BF16)
    c.QiT_d = dscr("QiT_d", [256, S], BF16)
    c.ZaT_d = dscr("ZaT_d", [512, S], BF16)
    c.ZbT_d = dscr("ZbT_d", [512, S], BF16)
    c.GT_d = dscr("GT_d", [24, S], BF16)
    with ExitStack() as es:
        def tsb(name, shape, dt=F32):
            return es.enter_context(nc.sbuf_tensor(name, list(shape), dt))
        wfm = tsb("wfm", [128, 8, FM0_COLS], BF16)
        wtm = tsb("wtm", [128, 8, TM0_COLS], BF16)
        hnT = tsb("hnT", [128, 8, S], BF16)
        gbc = tsb("gbc", [128, D], F32)
        xt = [tsb(f"xt{i}", [128, D], F32) for i in range(2)]
        hn = [tsb(f"hn{i}", [128, D], BF16) for i in range(2)]
        sq = tsb("sq", [128, D], BF16)
        st = [tsb(f"st{i}", [128, 4], F32) for i in range(2)]
        stage = [tsb(f"stage{i}", [128, 512], BF16) for i in range(3)]
        stage_f = tsb("stage_f", [128, 512], F32)
        b_wfm, b_wtm, b_g = Buf("wfm"), Buf("wtm"), Buf("gbc")
        b_hnT = bufs(NT, "hnT")
        b_xt, b_hn, b_st, b_sq = bufs(2, "xt"), bufs(2, "hn"), bufs(2, "st"), Buf("sq")
        b_stage, b_stage_f = bufs(3, "stage"), Buf("stagef")

        half = FM0_COLS // 2
        for kc in range(8):
            for h in range(2):
                sc.dma("pool", wfm[:, kc, h * half:(h + 1) * half],
                       c.w0_fm[kc * 128:(kc + 1) * 128, h * half:(h + 1) * half], writes=[b_wfm])
            sc.dma("pool", wtm[:, kc, :], c.w0_tm[kc * 128:(kc + 1) * 128, :], writes=[b_wtm])
        sc.dma("sp", gbc[:], c.norm_g0.partition_broadcast(128), writes=[b_g])
        sc.pool([], c.b_V, "memset", c.Vs[:, :, :, 64:65], 1.0)
        sc.pool([], c.b_V, "memset", c.Vw[:, :, :, 64:65], 1.0)
        sc.pool([], c.b_V, "memset", c.Vb[:, :, 64:65], 1.0)

        def p0(t):
            k = t % 2
            sc.dma("sp", xt[k][:], c.x[t * 128:(t + 1) * 128, :], writes=[b_xt[k]])
            sc.dve([b_xt[k]], [b_sq, b_st[k]], "scalar_tensor_tensor", out=sq[:], in0=xt[k][:], scalar=1.0 / D,
                   in1=xt[k][:], op0=ALU.mult, op1=ALU.mult, accum_out=st[k][:, 0:1])
            sc.act([b_st[k], c.b_const], [b_st[k]], "activation", out=st[k][:, 1:2], in_=st[k][:, 0:1], func=AF.Sqrt,
                   bias=c.eps_t[:, 0:1], scale=1.0)
            sc.dve([b_st[k]], [b_st[k]], "reciprocal", out=st[k][:, 2:3], in_=st[k][:, 1:2])
            sc.dve([b_xt[k], b_st[k], b_g], [b_hn[k]], "scalar_tensor_tensor", out=hn[k][:], in0=xt[k][:],
                   scalar=st[k][:, 2:3], in1=gbc[:], op0=ALU.mult, op1=ALU.mult)
            pbk = 6 + k
            tp = bank(c, pbk, 1, BF16)
            for kc in range(8):
                sc.pe([b_hn[k], c.b_const], [c.pb[pbk]], "transpose", out=tp[:, kc * 128:(kc + 1) * 128],
                      in_=hn[k][:, kc * 128:(kc + 1) * 128], identity=c.ident_b[:])
            sc.act([c.pb[pbk]], [b_hnT[t]], "activation", out=hnT[:, :, t * 128:(t + 1) * 128],
                   in_=tp.rearrange("p (c n) -> p c n", c=8), func=AF.Copy)

        nst = [0]

        def evac(kind, j, tg, ps, M, pbk):
            cols = slice(tg * 512, (tg + 1) * 512)
            if kind in ("qa", "qb", "qi"):
                scale = 0.125 if kind != "qi" else 1.0 / 16.0
                dst = {"qa": c.QaT_d, "qb": c.QbT_d, "qi": c.QiT_d}[kind]
                s = nst[0] % 3
                nst[0] += 1
                sc.dve([c.pb[pbk]], [b_stage[s]], "tensor_scalar", out=stage[s][:M, :], in0=ps, scalar1=scale,
                       scalar2=None, op0=ALU.mult)
                toks.append(sc.dma("sp", dst[j * 128:j * 128 + M, cols], stage[s][:M, :], reads=[b_stage[s]]))
            elif kind in ("za", "zb"):
                dst = {"za": c.ZaT_d, "zb": c.ZbT_d}[kind]
                s = nst[0] % 3
                nst[0] += 1
                sc.act([c.pb[pbk]], [b_stage[s]], "activation", out=stage[s][:M, :], in_=ps, func=AF.Silu)
                toks.append(sc.dma("sp", dst[j * 128:j * 128 + M, cols], stage[s][:M, :], reads=[b_stage[s]]))
            elif kind == "g":
                s = nst[0] % 3
                nst[0] += 1
                sc.act([c.pb[pbk]], [b_stage[s]], "activation", out=stage[s][:M, :], in_=ps, func=AF.Sigmoid)
                toks.append(sc.dma("sp", c.GT_d[0:M, cols], stage[s][:M, :], reads=[b_stage[s]]))
            else:
                dstt, dstb = {"kc": (c.KcT, c.b_KcT), "vc": (c.VcT, c.b_VcT), "ks": (c.KsT, c.b_KsT),
                              "kw": (c.KwT, c.b_KwT), "kbki": (c.KbKi, c.b_KbKi)}[kind]
                sc.dve([c.pb[pbk]], [dstb[tg]], "tensor_copy", out=dstt[:, cols], in_=ps)

        nb = 0
        for tg in range(NG):
            for t in range(tg * 4, tg * 4 + 4):
                p0(t)
            for t in range(tg * 4, tg * 4 + 4):
                pbk = 4 + (t % 2)
                ps = bank(c, pbk)[:, 0:TM0_COLS]
                for kc in range(8):
                    sc.pe([b_hnT[t], b_wtm], [c.pb[pbk]], "matmul", ps, lhsT=hnT[:, kc, t * 128:(t + 1) * 128],
                          rhs=wtm[:, kc, :], start=(kc == 0), stop=(kc == 7))
                sc.act([c.pb[pbk]], [c.b_V[t]], "activation", out=c.Vs[:, t, :, 0:64],
                       in_=ps[:, 0:128].rearrange("p (g d) -> p g d", g=2), func=AF.Copy)
                sc.act([c.pb[pbk]], [c.b_V[t]], "activation", out=c.Vw[:, t, :, 0:64],
                       in_=ps[:, 128:256].rearrange("p (g d) -> p g d", g=2), func=AF.Copy)
                sc.dve([c.pb[pbk]], [c.b_V[t]], "tensor_copy", out=c.Vb[:, t, 0:64], in_=ps[:, 256:320])
                sc.dve([c.pb[pbk]], [c.b_V[t]], "tensor_copy", out=c.WI[:, t, :], in_=ps[:, 320:324])
            col = 0
            for kind, ncols in FM0:
                nch = (ncols + 127) // 128
                for j in range(nch):
                    M = min(128, ncols - j * 128)
                    pbk = nb % 4
                    nb += 1
                    ps = bank(c, pbk)[:M, :]
                    for kc in range(8):
                        sc.pe([b_wfm] + b_hnT[tg * 4:tg * 4 + 4], [c.pb[pbk]], "matmul", ps,
                              lhsT=wfm[:, kc, col:col + M], rhs=hnT[:, kc, tg * 512:(tg + 1) * 512],
                              start=(kc == 0), stop=(kc == 7))
                    evac(kind, j, tg, ps, M, pbk)
                    col += M
    return toks


def _t5_bucket_np(d):
    n = np.maximum(d, 0)
    nf = np.maximum(n, 1).astype(np.float32)
    large = 16 + (np.log(nf / np.float32(16)) / np.float32(math.log(128 / 16)) * np.float32(16)).astype(np.int32)
    large = np.minimum(large, 31)
    return np.where(n < 16, n, large)


def host_consts(S, t5_table):
    NB = S // 64
    t = np.arange(S)
    cur = t // 64
    n = np.arange(NB)[None, :]
    forced = (n == 0) | (n == cur[:, None]) | (n == cur[:, None] - 1)
    F = np.where(n <= cur[:, None], 1e4 * forced, -1e30).astype(np.float32)
    tq = np.arange(128)
    d = tq[:, None] - 16 * np.arange(16)[None, :] + 113
    bc = np.where(d[:, None, :] >= 0, t5_table[_t5_bucket_np(d)][:, :, :8].transpose(0, 2, 1), NEGM)
    out = {"Fc": F, "Bc": np.ascontiguousarray(bc, np.float32)}
    for rel in (0, 1):
        d = tq[None, :] - tq[:, None] + 128 * rel
        tb = t5_table[_t5_bucket_np(d)]
        tb = np.where(d[:, :, None] >= 0, tb, NEGM).transpose(0, 2, 1)
        out[f"AB{rel}"] = np.ascontiguousarray(tb[:, :8, :], np.float32)
        out[f"DB{rel}"] = np.ascontiguousarray(tb[:, 8:, :], np.float32)
    w4 = np.where(tq[:, None] <= tq[None, :], NEGM, 0.0).astype(np.float32)
    out["W4"] = np.ascontiguousarray(np.broadcast_to(w4[:, None, :], (128, 4, 128)))
    out["tb31"] = np.ascontiguousarray(t5_table[31:32, :], np.float32)
    out["I4"] = np.ascontiguousarray(np.tile(np.eye(128, dtype=np.float32), (1, 4)))
    return out


def layer0_cmp(c):
    nc, sc, S = c.nc, c.sc, c.S
    n_cmp = S // 16 - 1
    NCP = S // 16 + 1
    NCOL = S // 16
    NCC = (n_cmp + 127) // 128
    c.n_cmp, c.NCOL, c.NCC = n_cmp, NCOL, NCC
    c.b_cmp = Buf("cmp")
    w1k_d = c.din("cmp_w1_k", [2048, 256])
    w1v_d = c.din("cmp_w1_v", [2048, 256])
    w2k_d = c.din("cmp_w2_k", [256, 64])
    w2v_d = c.din("cmp_w2_v", [256, 64])
    posk_d = c.din("cmp_posT_k", [128, 32])
    posv_d = c.din("cmp_posT_v", [128, 32])
    with ExitStack() as es:
        def tsb(name, shape, dt=F32):
            return es.enter_context(nc.sbuf_tensor(name, list(shape), dt))
        w1 = [tsb(f"w1_{i}", [128, 32, 256], BF16) for i in range(2)]
        w2kp = tsb("w2kp", [128, 2, 2, 128], BF16)
        w2v = tsb("w2v", [128, 2, 64], BF16)
        hid = tsb("hid", [128, 2, 2, 2, NCOL], BF16)
        bia = tsb("bia", [128, 8], F32)
        b_w, b_hid, b_bia = Buf("cmpw"), bufs(8, "hid"), Buf("bia")
        for i, wd in enumerate((w1k_d, w1v_d)):
            src = wd.rearrange("(l d) j -> d l j", d=64)
            for half in range(2):
                for lh in range(2):
                    sc.dma("pool", w1[i][half * 64:(half + 1) * 64, lh * 16:(lh + 1) * 16, :], src[:, lh * 16:(lh + 1) * 16, :], writes=[b_w])
        sc.pool([], [b_w], "memset", w2kp[:], 0.0)
        for g in range(2):
            sc.dma("pool", w2kp[:, :, g, g * 64:(g + 1) * 64], w2k_d.rearrange("(c p) d -> p c d", p=128), writes=[b_w])
        sc.dma("pool", w2v[:], w2v_d.rearrange("(c p) d -> p c d", p=128), writes=[b_w])
        sc.dma("pool", c.KcT[:, S:S + 32], posk_d, writes=[c.b_KcT[-1]])
        sc.dma("pool", c.VcT[:, S:S + 32], posv_d, writes=[c.b_VcT[-1]])
        n = 0
        for kv, (src, bsrc) in enumerate(((c.KcT, c.b_KcT), (c.VcT, c.b_VcT))):
            for g in range(2):
                for jc in range(2):
                    pbk = n % 4
                    ps = bank(c, pbk)[:, 0:NCP]
                    for l in range(32):
                        sc.pe(bsrc + [b_w], [c.pb[pbk]], "matmul", ps,
                              lhsT=w1[kv][g * 64:(g + 1) * 64, l, jc * 128:(jc + 1) * 128],
                              rhs=src[g * 64:(g + 1) * 64, l:l + 16 * (NCP - 1) + 1:16], start=(l == 0), stop=(l == 31))
                    sc.dve([c.pb[pbk]], [b_bia], "tensor_copy", out=bia[:, n:n + 1], in_=ps[:, NCP - 1:NCP])
                    sc.act([c.pb[pbk], b_bia], [b_hid[n]], "activation", out=hid[:, kv, g, jc, 0:n_cmp], in_=ps[:, 0:n_cmp],
                           func=AF.Silu, bias=bia[:, n:n + 1], scale=1.0)
                    n += 1
        ps = bank(c, 4)[:, 0:n_cmp]
        k = 0
        for g in range(2):
            for jc in range(2):
                sc.pe(b_hid + [b_w], [c.pb[4]], "matmul", ps, lhsT=w2kp[:, jc, g, :], rhs=hid[:, 0, g, jc, 0:n_cmp],
                      start=(k == 0), stop=(k == 3))
                k += 1
        sc.dve([c.pb[4]], [c.b_cmp], "tensor_copy", out=c.KcmpT[:, 0:n_cmp], in_=ps)
        for g in range(2):
            for cc in range(NCC):
                rows = min(128, n_cmp - cc * 128)
                pbk = 5 + (g * NCC + cc) % 2
                ps = bank(c, pbk)[0:rows, 0:64]
                for jc in range(2):
                    sc.pe(b_hid + [b_w], [c.pb[pbk]], "matmul", ps, lhsT=hid[:, 1, g, jc, cc * 128:cc * 128 + rows],
                          rhs=w2v[:, jc, :], start=(jc == 0), stop=(jc == 1))
                sc.dve([c.pb[pbk]], [c.b_cmp], "tensor_copy", out=c.Vcmp[0:rows, cc, g, :], in_=ps)
    return []


def layer0_attn(c, x1_d):
    nc, sc, S, NT = c.nc, c.sc, c.S, c.NT
    NB = S // 64
    NCOL, NCC, n_cmp = c.NCOL, c.NCC, c.n_cmp
    toks = []
    hc = c.hc
    wout_d = c.din("w_out0", [D, D])
    gw_d = c.din("ple_gw0", [D, D])
    plew_d = c.din("ple_w0", [256, D])
    p_d = c.din("p0", [S, 256])
    es = ExitStack()

    def tsb(name, shape, dt=F32):
        return es.enter_context(nc.sbuf_tensor(name, list(shape), dt))

    b_w = Buf("attw")
    wout = tsb("wout", [128, 8, D], BF16)
    Y2 = tsb("Y2", [128, 8, 128], BF16)
    b_Y2 = Buf("Y2")
    gw = tsb("gw", [128, 8, D], BF16)
    plew = tsb("plew", [128, 2, D], BF16)
    for kc in range(8):
        sc.dma("pool", wout[:, kc, :], wout_d[kc * 128:(kc + 1) * 128, :], writes=[b_w])
    for kc in range(8):
        sc.dma("pool", gw[:, kc, :], gw_d[kc * 128:(kc + 1) * 128, :], writes=[b_w])
    for kc in range(2):
        sc.dma("pool", plew[:, kc, :], plew_d[kc * 128:(kc + 1) * 128, :], writes=[b_w])
    Bc = tsb("Bc", [128, 8, 16], BF16)
    AB = [tsb(f"AB{r}", [128, 8, 128], BF16) for r in range(2)]
    DB = [tsb(f"DB{r}", [128, 8, 128], BF16) for r in range(2)]
    W4 = tsb("W4", [128, 4, 128], BF16)
    IDX = tsb("IDX", [128, S])
    stg = IDX[:, 0:1024].rearrange("p (h t) -> p h t", h=8)
    tb31 = tsb("tb31", [128, 16])
    I4 = tsb("I4", [128, 512], BF16)
    ones_f = tsb("ones_f", [128, 64])
    b_k = Buf("attc")
    sc.dma("sp", tb31[:], hc["tb31"].partition_broadcast(128), writes=[b_k])
    sc.dma("pool", W4[:], hc["W4"], writes=[b_k])
    sc.dma("sp", stg[:, :, 0:16], hc["Bc"], writes=[b_k])
    sc.dve([b_k], [b_k], "tensor_tensor", out=Bc[:], in0=stg[:, :, 0:16], in1=tb31[:, 0:8].unsqueeze(2).to_broadcast([128, 8, 16]), op=ALU.subtract)
    for r in range(2):
        sc.dma("sp", stg[:], hc[f"AB{r}"], writes=[b_k])
        sc.dve([b_k], [b_k], "tensor_tensor", out=AB[r][:], in0=stg[:], in1=tb31[:, 0:8].unsqueeze(2).to_broadcast([128, 8, 128]), op=ALU.subtract)
        sc.dma("sp", stg[:], hc[f"DB{r}"], writes=[b_k])
        sc.dve([b_k], [b_k], "tensor_tensor", out=DB[r][:], in0=stg[:], in1=tb31[:, 8:16].unsqueeze(2).to_broadcast([128, 8, 128]), op=ALU.subtract)
    sc.dma("pool", I4[:], hc["I4"], writes=[b_k])
    sc.dve([], [b_k], "memset", ones_f[:], 1.0)

    qa_t = tsb("qa_t", [128, 4, 128], BF16)
    qb_t = tsb("qb_t", [64, 8, 128], BF16)
    qi_t = tsb("qi_t", [128, 4, 128], BF16)
    za_t = tsb("za_t", [64, 8, 128], BF16)
    zb_t = tsb("zb_t", [64, 8, 128], BF16)
    g_t = tsb("g_t", [64, 24, 128], BF16)
    x_t = tsb("x_t", [128, D])
    p_t = tsb("p_t", [128, 256])
    F_t = tsb("F_t", [128, NB])
    b_q, b_z, b_g, b_x, b_p, b_F = Buf("q"), Buf("z"), Buf("g"), Buf("x"), Buf("p"), Buf("F")
    E = tsb("E", [128, 8, NCOL])
    P = E
    PS = tsb("PS", [128, 2, NCOL])
    IMP = tsb("IMP", [128, 2, NB])
    SCR = tsb("SCR", [128, 2, NB])
    T8 = tsb("T8", [128, 2, 8])
    den = tsb("den", [128, 16])
    MKs = [tsb(f"MK{g}", [128, S], BF16) for g in range(2)]
    b_MKs = bufs(2, "MK")
    PcT_flat = tsb("PcT", [128, NCC * 2 * 512], BF16)
    PcT = PcT_flat[:].rearrange("p (c g n) -> p c g n", c=NCC, g=2)
    OC = tsb("OC", [64, 8, 128])
    b_E, b_PS, b_IMP, b_SCR, b_T8, b_den, b_PcT, b_OC = (Buf(n) for n in "E PS IMP SCR T8 den PcT OC".split())
    b_P = b_E
    PT = [tsb(f"PT{i}", [128, 2, 512], BF16) for i in range(2)]
    b_PT = bufs(2, "PT")
    OS = tsb("OS", [65, 2, 512])
    OW = tsb("OW", [65, 2, 512])
    OB = tsb("OB", [65, 2, 512])
    b_OS, b_OW, b_OB = Buf("OS"), Buf("OW"), Buf("OB")
    R = tsb("R", [128, 1024])
    MD = tsb("MD", [128, S], BF16)
    bs = tsb("bs", [128, 8])
    bsi = tsb("bsi", [128, 2], I32)
    b_IDX, b_R, b_bs = Buf("IDX"), Buf("R"), Buf("bs")
    b_MD = Buf("MD")
    t1 = R[0:64, 0:512]
    t2 = R[0:64, 512:1024]
    t3 = PcT_flat[0:64, 0:1024].bitcast(F32)
    b_t1, b_t2, b_t3 = b_R, b_R, b_PcT
    YA = tsb("YA", [64, 8, 128], BF16)
    YB = tsb("YB", [64, 8, 128], BF16)
    b_Y = Buf("Y")
    XA = x_t
    XAb = tsb("XAb", [128, D], BF16)
    XAT = tsb("XAT", [128, 8, 128], BF16)
    Pb = tsb("Pb", [128, 256], BF16)
    PTT = tsb("PTT", [128, 2, 128], BF16)
    SG = tsb("SG", [128, D])
    XB = SG
    b_XAb, b_XAT, b_Pb, b_PTT, b_SG = (Buf(n) for n in "XAb XAT Pb PTT SG".split())
    b_XA, b_XB = b_x, b_SG

    sc.dve([], [b_P], "memset", P[:], 0.0)
    sc.pool([], [b_PS], "memset", PS[:], 0.0)

    pb = c.pb
    allK = c.b_KsT + c.b_KwT + c.b_KbKi
    grp = [0]

    def attn_units(i, js, kT, kbase, kb_bufs, q_rhs, b_qr, mask, b_mask, bias_fn, V_fn, b_V, acc_bank, first):
        acc = bank(c, acc_bank)[0:65, :]
        n = len(js)
        for u0 in range(0, n, 2):
            us = js[u0:u0 + 2]
            k = grp[0] % 2
            grp[0] += 1
            b0 = 2 * k
            for ui, j in enumerate(us):
                ps = bank(c, b0 + ui)
                rel = i - j
                bias = bias_fn(rel)
                last_mm = "qk"
                if bias is not None:
                    last_mm = "bias"
                elif mask is not None:
                    last_mm = "mask"
                sc.pe(kb_bufs + [b_qr], [pb[b0 + ui]], "matmul", ps, lhsT=kT[kbase:kbase + 64, j * 128:(j + 1) * 128], rhs=q_rhs,
                      start=True, stop=(last_mm == "qk"))
                if mask is not None:
                    sc.pe([b_mask, b_k], [pb[b0 + ui]], "matmul", ps, lhsT=mask[:, j * 128:(j + 1) * 128], rhs=I4[:],
                          start=False, stop=(last_mm == "mask"))
                if bias is not None:
                    sc.pe([b_k, c.b_const], [pb[b0 + ui]], "matmul", ps, lhsT=c.ident_b[:], rhs=bias, start=False, stop=True)
            nu = len(us)
            sc.act([pb[b0 + ui] for ui in range(nu)], [b_PT[k]], "activation", out=PT[k][:, 0:nu, :],
                   in_=bank(c, b0, nu).rearrange("p (u n) -> p u n", u=nu), func=AF.Exp)
            for ui, j in enumerate(us):
                sc.pe([b_PT[k], b_V[j]], [pb[acc_bank]], "matmul", acc, lhsT=V_fn(j), rhs=PT[k][:, ui, :],
                      start=(first and j == js[0]), stop=(j == js[-1]))

    def part_A1a(i):
        cols = slice(i * 128, (i + 1) * 128)
        N_i = (i + 1) * 128
        N_c = min(n_cmp, 8 * i + 7)
        sc.dma("sp", qa_t[:], c.QaT_d.rearrange("(m p) s -> p m s", p=128)[:, :, cols], writes=[b_q])
        sc.dma("sp", qb_t[:], c.QbT_d.rearrange("(h d) s -> d h s", d=64)[:, :, cols], writes=[b_q])
        sc.dma("sp", qi_t[64:128, :, :], c.QiT_d.rearrange("(h d) s -> d h s", d=64)[:, :, cols], writes=[b_q])
        sc.dma("sp", za_t[:], c.ZaT_d.rearrange("(h d) s -> d h s", d=64)[:, :, cols], writes=[b_z])
        sc.dma("sp", zb_t[:], c.ZbT_d.rearrange("(h d) s -> d h s", d=64)[:, :, cols], writes=[b_z])
        sc.dma("sp", g_t[:], bass.AP(tensor=c.GT_d.tensor, offset=i * 128, ap=[[0, 64], [S, 24], [1, 128]]), writes=[b_g])
        sc.dma("sp", F_t[:], hc["Fc"][cols, :], writes=[b_F])

        N_c = min(n_cmp, 8 * i + 7)
        Sc = bank(c, 0, 4).rearrange("p (h n) -> p h n", h=8)
        c_lo = max(0, 8 * i - 9)
        c_hi = min(N_c, 8 * i + 7)
        j_lo = c_lo - (8 * i - 9)
        for h in range(8):
            g, hh = divmod(h, 4)
            pbk = h // 2
            sc.pe([b_q, c.b_cmp], [pb[pbk]], "matmul", Sc[:, h, 0:N_c], lhsT=qa_t[g * 64:(g + 1) * 64, hh, :],
                  rhs=c.KcmpT[g * 64:(g + 1) * 64, 0:N_c], start=True, stop=False)
            sc.pe([b_k, c.b_const], [pb[pbk]], "matmul", Sc[:, h, c_lo:c_hi], lhsT=c.ident_b[:],
                  rhs=Bc[:, h, j_lo:j_lo + (c_hi - c_lo)], start=False, stop=True)
        for h in range(8):
            sc.act([pb[h // 2]], [b_E, b_den], "activation", out=E[:, h, 0:N_c], in_=Sc[:, h, 0:N_c], func=AF.Exp,
                   accum_out=den[:, h:h + 1])
        for k0 in range(0, N_i, 1024):
            kn = min(1024, N_i - k0)
            nbk = (kn + 511) // 512
            for hh in range(4):
                k = grp[0] % 2
                grp[0] += 1
                b0 = 2 * k
                for bq in range(nbk):
                    w = min(512, kn - bq * 512)
                    sc.pe([b_q] + c.b_KbKi, [pb[b0 + bq]], "matmul", bank(c, b0 + bq)[:, 0:w], lhsT=qi_t[64:128, hh, :],
                          rhs=c.KbKi[64:128, k0 + bq * 512:k0 + bq * 512 + w], start=True, stop=True)
                if hh == 0:
                    sc.act([pb[b0 + bq] for bq in range(nbk)], [b_R], "activation", out=R[:, 0:kn], in_=bank(c, b0, 2)[:, 0:kn], func=AF.Relu)
                    sc.dve([b_R, c.b_V[i]], [b_IDX], "tensor_scalar", out=IDX[:, k0:k0 + kn], in0=R[:, 0:kn], scalar1=c.WI[:, i, 0:1],
                           scalar2=None, op0=ALU.mult)
                else:
                    sc.act([pb[b0 + bq] for bq in range(nbk)], [b_R], "activation", out=R[:, 0:kn], in_=bank(c, b0, 2)[:, 0:kn], func=AF.Relu)
                    sc.dve([b_R, c.b_V[i], b_IDX], [b_IDX], "scalar_tensor_tensor", out=IDX[:, k0:k0 + kn], in0=R[:, 0:kn],
                           scalar=c.WI[:, i, hh:hh + 1], in1=IDX[:, k0:k0 + kn], op0=ALU.mult, op1=ALU.add)

    def part_A1b(i):
        cols = slice(i * 128, (i + 1) * 128)
        N_i = (i + 1) * 128
        N_c = min(n_cmp, 8 * i + 7)
        sc.dve([b_den], [b_den], "tensor_scalar", out=den[:, 8:16], in0=den[:, 0:8], scalar1=1e-30, scalar2=None, op0=ALU.max)
        sc.dve([b_den], [b_den], "reciprocal", out=den[:, 8:16], in_=den[:, 8:16])
        sc.dve([b_E, b_den], [b_P], "tensor_tensor", out=P[:, :, 0:N_c], in0=E[:, :, 0:N_c],
               in1=den[:, 8:16].unsqueeze(2).to_broadcast([128, 8, N_c]), op=ALU.mult)
        sc.dve([b_P], [b_PS], "tensor_reduce", out=PS[:, :, 0:N_c], in_=P[:, :, 0:N_c].rearrange("p (g h) n -> p g n h", g=2),
               axis=AX.X, op=ALU.add)
        sc.dve([b_PS], [b_IMP], "tensor_reduce", out=IMP[:], in_=PS[:].rearrange("p g (n r) -> p g n r", r=4), axis=AX.X, op=ALU.add)
        sc.dve([b_PS, b_IMP], [b_IMP], "tensor_tensor", out=IMP[:, :, 1:NB], in0=IMP[:, :, 1:NB], in1=PS[:, :, 3:4 * NB - 1:4], op=ALU.add)
        sc.dve([b_IMP, b_F], [b_SCR], "tensor_tensor", out=SCR[:], in0=IMP[:], in1=F_t[:].unsqueeze(1).to_broadcast([128, 2, NB]), op=ALU.add)
        for g in range(2):
            sc.dve([b_SCR], [b_T8], "max", out=T8[:, g, :], in_=SCR[:, g, :])
        for g in range(2):
            MK, b_MK = MKs[g], b_MKs[g]
            nblk = 2 * (i + 1)
            sc.dve([b_SCR, b_T8], [b_MK], "tensor_scalar", out=MK[:, 0:N_i].rearrange("p (n k) -> p n k", k=64),
                   in0=SCR[:, g, 0:nblk].unsqueeze(2).to_broadcast([128, nblk, 64]), scalar1=T8[:, g, 7:8], scalar2=NEGM,
                   op0=ALU.is_lt, op1=ALU.mult)
            sc.pool([b_MK], [b_MK], "affine_select", out=MK[:, cols], in_=MK[:, cols], pattern=[[-1, 128]],
                    compare_op=ALU.is_ge, fill=NEGM, base=0, channel_multiplier=1)

    def part_A1c(i):
        cols = slice(i * 128, (i + 1) * 128)
        N_i = (i + 1) * 128
        N_c = min(n_cmp, 8 * i + 7)
        sc.pool([b_IDX], [b_IDX], "affine_select", out=IDX[:, cols], in_=IDX[:, cols], pattern=[[-1, 128]],
                compare_op=ALU.is_ge, fill=-1e30, base=0, channel_multiplier=1)
        if i < 2:
            sc.dve([], [b_bs], "memset", bs[:, 2:3], -1e29)
        else:
            sub = IDX[:, 0:i * 128]
            sc.dve([b_IDX], [b_bs], "tensor_reduce", out=bs[:, 0:1], in_=sub, axis=AX.X, op=ALU.max)
            sc.dve([b_IDX], [b_bs], "tensor_reduce", out=bs[:, 2:3], in_=sub, axis=AX.X, op=ALU.min)
            sc.dve([b_bs], [b_bs], "tensor_tensor", out=bs[:, 1:2], in0=bs[:, 0:1], in1=bs[:, 2:3], op=ALU.subtract)
            for it in range(1, 21):
                sc.dve([b_bs], [b_bs], "scalar_tensor_tensor", out=bs[:, 3:4], in0=bs[:, 1:2], scalar=2.0 ** (-it), in1=bs[:, 2:3],
                       op0=ALU.mult, op1=ALU.add)
                sc.dve([b_bs, b_IDX], [b_MD, b_bs], "tensor_scalar", out=MD[:, 0:N_i], in0=IDX[:, 0:N_i], scalar1=bs[:, 3:4], scalar2=None,
                       op0=ALU.is_ge, op1=ALU.add, accum_out=bs[:, 4:5])
                sc.dve([b_bs], [b_bs], "tensor_scalar", out=bsi[:, 0:1], in0=bs[:, 4:5], scalar1=255.5, scalar2=None, op0=ALU.is_gt)
                sc.dve([b_bs], [b_bs], "copy_predicated", out=bs[:, 2:3], mask=bsi[:, 0:1], data=bs[:, 3:4])
        sc.dve([b_bs, b_IDX], [b_MD], "tensor_scalar", out=MD[:, 0:N_i], in0=IDX[:, 0:N_i], scalar1=bs[:, 2:3], scalar2=NEGM,
               op0=ALU.is_lt, op1=ALU.mult)


    def part_A2(i):
        cols = slice(i * 128, (i + 1) * 128)
        N_i = (i + 1) * 128
        N_c = min(n_cmp, 8 * i + 7)
        ncc = (N_c + 127) // 128
        for g in range(2):
            for cc in range(ncc):
                rows = min(128, N_c - cc * 128)
                pbk = 4 + (g * ncc + cc) % 2
                for hh in range(4):
                    sc.pe([b_P, c.b_const], [pb[pbk]], "transpose", out=bank(c, pbk)[0:rows, hh * 128:(hh + 1) * 128],
                          in_=P[:, g * 4 + hh, cc * 128:cc * 128 + rows], identity=c.ident_f[:])
                sc.act([pb[pbk]], [b_PcT], "activation", out=PcT[0:rows, cc, g, :], in_=bank(c, pbk)[0:rows, :], func=AF.Copy)
        for g in range(2):
            pbk = 6 + g
            for cc in range(ncc):
                rows = min(128, N_c - cc * 128)
                sc.pe([b_PcT, c.b_cmp], [pb[pbk]], "matmul", bank(c, pbk)[0:64, :], lhsT=c.Vcmp[0:rows, cc, g, :],
                      rhs=PcT[0:rows, cc, g, :], start=(cc == 0), stop=(cc == ncc - 1))
            sc.act([pb[pbk]], [b_OC], "activation", out=OC[:, g * 4:(g + 1) * 4, :],
                   in_=bank(c, pbk)[0:64, :].rearrange("p (h t) -> p h t", h=4), func=AF.Copy)


    def part_B(i):
        cols = slice(i * 128, (i + 1) * 128)
        N_i = (i + 1) * 128
        N_c = min(n_cmp, 8 * i + 7)
        for g in range(2):
            MK, b_MK = MKs[g], b_MKs[g]
            q_rhs = qa_t[g * 64:(g + 1) * 64, :, :]
            ab = lambda rel, g=g: (AB[rel][:, g * 4:(g + 1) * 4, :] if rel in (0, 1) else None)
            attn_units(i, list(range(0, i + 1)), c.KsT, g * 64, c.b_KsT, q_rhs, b_q, MK, b_MK, ab,
                       lambda j, g=g: c.Vs[:, j, g, :], c.b_V, 4 + g, True)
            sc.act([pb[4 + g]], [b_OS], "activation", out=OS[:, g, :], in_=bank(c, 4 + g)[0:65, :], func=AF.Copy)
            wb = lambda rel, g=g: (AB[rel][:, g * 4:(g + 1) * 4, :] if rel in (0, 1) else (W4[:] if rel == 4 else None))
            attn_units(i, list(range(max(0, i - 4), i + 1)), c.KwT, g * 64, c.b_KwT, q_rhs, b_q, None, None, wb,
                       lambda j, g=g: c.Vw[:, j, g, :], c.b_V, 4 + g, True)
            sc.act([pb[4 + g]], [b_OW], "activation", out=OW[:, g, :], in_=bank(c, 4 + g)[0:65, :], func=AF.Copy)

        for hg in range(2):
            db = lambda rel, hg=hg: (DB[rel][:, hg * 4:(hg + 1) * 4, :] if rel in (0, 1) else None)
            attn_units(i, list(range(0, i + 1)), c.KbKi, 0, c.b_KbKi, qb_t[:, hg * 4:(hg + 1) * 4, :], b_q, MD, b_MD, db,
                       lambda j: c.Vb[:, j, :], c.b_V, 4 + hg, True)
            sc.act([pb[4 + hg]], [b_OB], "activation", out=OB[:, hg, :], in_=bank(c, 4 + hg)[0:65, :], func=AF.Copy)

        for bi, (O, b_O) in enumerate(((OS, b_OS), (OW, b_OW), (OB, b_OB))):
            sc.act([b_O], [b_O], "activation", out=O[64:65, :, :], in_=O[64:65, :, :], func=AF.Ln)
            sc.act([b_O], [b_O], "activation", out=O[64:65, :, :], in_=O[64:65, :, :], func=AF.Exp, scale=-1.0)
        for g in range(2):
            hs = slice(g * 4, (g + 1) * 4)
            sc.pe([b_OS, b_k], [pb[6]], "matmul", bank(c, 6)[0:64, :], lhsT=ones_f[64:65, 0:64], rhs=OS[64:65, g, :], start=True, stop=True)
            sc.dve([b_OS, pb[6]], [b_t1], "tensor_tensor", out=t1[:], in0=OS[0:64, g, :], in1=bank(c, 6)[0:64, :], op=ALU.mult)
            sc.pool([b_t1, b_g], [b_t1], "tensor_tensor", out=t1[:].rearrange("p (h t) -> p h t", h=4), in0=t1[:].rearrange("p (h t) -> p h t", h=4),
                    in1=g_t[:, g * 12 + 1:g * 12 + 12:3, :], op=ALU.mult)
            sc.pe([b_OW, b_k], [pb[7]], "matmul", bank(c, 7)[0:64, :], lhsT=ones_f[64:65, 0:64], rhs=OW[64:65, g, :], start=True, stop=True)
            sc.dve([b_OW, pb[7]], [b_t2], "tensor_tensor", out=t2[:], in0=OW[0:64, g, :], in1=bank(c, 7)[0:64, :], op=ALU.mult)
            sc.pool([b_t2, b_g], [b_t2], "tensor_tensor", out=t2[:].rearrange("p (h t) -> p h t", h=4), in0=t2[:].rearrange("p (h t) -> p h t", h=4),
                    in1=g_t[:, g * 12 + 2:g * 12 + 12:3, :], op=ALU.mult)
            sc.pool([b_OC, b_g], [b_t3], "tensor_tensor", out=t3[:].rearrange("p (h t) -> p h t", h=4), in0=OC[:, hs, :],
                    in1=g_t[:, g * 12 + 0:g * 12 + 12:3, :], op=ALU.mult)
            sc.dve([b_t1, b_t2], [b_t1], "tensor_tensor", out=t1[:], in0=t1[:], in1=t2[:], op=ALU.add)
            sc.dve([b_t1, b_t3], [b_t1], "tensor_tensor", out=t1[:], in0=t1[:], in1=t3[:], op=ALU.add)
            sc.dve([b_t1, b_z], [b_Y], "tensor_tensor", out=YA[:, hs, :], in0=t1[:].rearrange("p (h t) -> p h t", h=4), in1=za_t[:, hs, :], op=ALU.mult)
            sc.pe([b_OB, b_k], [pb[6]], "matmul", bank(c, 6)[0:64, :], lhsT=ones_f[64:65, 0:64], rhs=OB[64:65, g, :], start=True, stop=True)
            sc.dve([b_OB, pb[6]], [b_t2], "tensor_tensor", out=t2[:], in0=OB[0:64, g, :], in1=bank(c, 6)[0:64, :], op=ALU.mult)
            sc.dve([b_t2, b_z], [b_Y], "tensor_tensor", out=YB[:, hs, :], in0=t2[:].rearrange("p (h t) -> p h t", h=4), in1=zb_t[:, hs, :], op=ALU.mult)


    def part_T(i):
        cols = slice(i * 128, (i + 1) * 128)
        N_i = (i + 1) * 128
        N_c = min(n_cmp, 8 * i + 7)
        sc.dma("sp", x_t[:], c.x[cols, :], writes=[b_x])
        sc.dma("sp", p_t[:], p_d[cols, :], writes=[b_p])
        for yi, Ysrc in enumerate((YA, YB)):
            for r in range(2):
                sc.dma("sp", Y2[r * 64:(r + 1) * 64, yi * 4:(yi + 1) * 4, :], Ysrc[:, r:8:2, :], reads=[b_Y], writes=[b_Y2])
        for half in range(2):
            for q in range(8):
                sc.pe([b_Y2, b_w], [pb[6 + half]], "matmul", bank(c, 6 + half), lhsT=Y2[:, q, :], rhs=wout[:, q, half * 512:(half + 1) * 512],
                      start=(q == 0), stop=(q == 7))
        sc.dve([b_x, pb[6], pb[7]], [b_XA], "tensor_tensor", out=XA[:], in0=x_t[:], in1=bank(c, 6, 2), op=ALU.add)
        sc.act([b_XA], [b_XAb], "activation", out=XAb[:], in_=XA[:], func=AF.Copy)
        sc.act([b_p], [b_Pb], "activation", out=Pb[:], in_=p_t[:], func=AF.Copy)
        tp = bank(c, 6, 1, BF16)
        for kc in range(8):
            sc.pe([b_XAb, c.b_const], [pb[6]], "transpose", out=tp[:, kc * 128:(kc + 1) * 128], in_=XAb[:, kc * 128:(kc + 1) * 128], identity=c.ident_b[:])
        sc.dve([pb[6]], [b_XAT], "tensor_copy", out=XAT[:], in_=tp.rearrange("p (c n) -> p c n", c=8))
        tp2 = bank(c, 7, 1, BF16)
        for kc in range(2):
            sc.pe([b_Pb, c.b_const], [pb[7]], "transpose", out=tp2[:, kc * 128:(kc + 1) * 128], in_=Pb[:, kc * 128:(kc + 1) * 128], identity=c.ident_b[:])
        sc.dve([pb[7]], [b_PTT], "tensor_copy", out=PTT[:], in_=tp2[:, 0:256].rearrange("p (c n) -> p c n", c=2))
        for half in range(2):
            for kc in range(8):
                sc.pe([b_XAT, b_w], [pb[4 + half]], "matmul", bank(c, 4 + half), lhsT=XAT[:, kc, :], rhs=gw[:, kc, half * 512:(half + 1) * 512],
                      start=(kc == 0), stop=(kc == 7))
        sc.act([pb[4], pb[5]], [b_SG], "activation", out=SG[:], in_=bank(c, 4, 2), func=AF.Sigmoid)
        for half in range(2):
            for kc in range(2):
                sc.pe([b_PTT, b_w], [pb[6 + half]], "matmul", bank(c, 6 + half), lhsT=PTT[:, kc, :], rhs=plew[:, kc, half * 512:(half + 1) * 512],
                      start=(kc == 0), stop=(kc == 1))
        sc.dve([b_SG, pb[6], pb[7]], [b_XB], "tensor_tensor", out=XB[:], in0=SG[:], in1=bank(c, 6, 2), op=ALU.mult)
        sc.dve([b_XB, b_XA], [b_XB], "tensor_tensor", out=XB[:], in0=XB[:], in1=XA[:], op=ALU.add)
        toks.append(sc.dma("sp", x1_d[cols, :], XB[:], reads=[b_XB]))

    for i in range(NT):
        part_A1a(i)
        if i > 0:
            part_T(i - 1)
        part_A1b(i)
        part_A2(i)
        part_A1c(i)
        part_B(i)
    part_T(NT - 1)
    es.close()
    return toks


def shared_inputs(inputs, S):
    f = lambda a: np.ascontiguousarray(np.asarray(a, dtype=np.float32))
    t5 = f(inputs["t5_table"])
    wfm, wtm = _perm_attn_w_in(f(inputs["attn_w_in"][0]))
    sh = {
        "norm_g0": f(inputs["norm_g"][0][None, :]),
        "w0_fm": wfm, "w0_tm": wtm,
        "ident": np.eye(128, dtype=np.float32),
        "cmp_w1_k": f(inputs["cmp_w1_k"][0]), "cmp_w1_v": f(inputs["cmp_w1_v"][0]),
        "cmp_w2_k": f(inputs["cmp_w2_k"][0]), "cmp_w2_v": f(inputs["cmp_w2_v"][0]),
        "cmp_posT_k": f(np.tile(np.asarray(inputs["cmp_pos_k"][0]).T, (2, 1))),
        "cmp_posT_v": f(np.tile(np.asarray(inputs["cmp_pos_v"][0]).T, (2, 1))),
        "w_out0": f(inputs["attn_w_out"][0]),
        "ple_gw0": f(inputs["ple_gate_w"][0]),
        "ple_w0": f(inputs["ple_w"][0]),
    }
    for k, v in host_consts(S, t5).items():
        sh["hc_" + k] = v
    w1fm, w1tm = _perm_rec_w_in(f(inputs["rec_w_in"][0]))
    sh["w1_fm"], sh["w1_tm"] = w1fm, w1tm
    sh["norm_g1"] = f(inputs["norm_g"][1][None, :])
    sh["final_g"] = f(np.asarray(inputs["final_g"])[None, :])
    cw = np.asarray(inputs["lru_conv_w"][0])
    vec = np.stack([cw[0], cw[1], cw[2], cw[3], np.asarray(inputs["lru_conv_b"][0]), np.asarray(inputs["lru_ba"][0]),
                    np.asarray(inputs["lru_bx"][0]), np.asarray(inputs["lru_lambda"][0])], axis=-1)
    sh["lru_vec"] = f(vec.reshape(4, 128, 8).transpose(1, 0, 2))
    for nm, key in (("lru_wa_bd", "lru_wa"), ("lru_wx_bd", "lru_wx")):
        w = np.asarray(inputs[key][0])
        bd = np.zeros((4, 128, 128), np.float32)
        for m in range(4):
            bd[m, 0:64, 0:64] = w[2 * m]
            bd[m, 64:128, 64:128] = w[2 * m + 1]
        sh[nm] = bd
    mw = np.asarray(inputs["mlstm_conv_w"][0])
    mv = np.stack([mw[0], mw[1], mw[2], mw[3], np.asarray(inputs["mlstm_conv_b"][0])], axis=-1)
    sh["ml_vec"] = f(mv.reshape(8, 128, 5).transpose(1, 0, 2))
    sh["ml_b"] = f(np.concatenate([np.asarray(inputs["mlstm_b_i"][0]), np.asarray(inputs["mlstm_b_f"][0])])[None, :])
    l1c = layer1_consts()
    sh["l1_tri"], sh["l1_e63"] = l1c["tri"], l1c["e63"]
    sh["w_out1"] = f(inputs["rec_w_out"][0])
    sh["ple_gw1"] = f(inputs["ple_gate_w"][1])
    sh["ple_w1"] = f(inputs["ple_w"][1])
    return sh


def core_inputs(inputs, b, S, shared=None):
    if shared is None:
        shared = shared_inputs(inputs, S)
    im = dict(shared)
    im["x"] = np.ascontiguousarray(np.asarray(inputs["x"][b, :S], dtype=np.float32))
    im["p0"] = np.ascontiguousarray(np.asarray(inputs["p"][0, b, :S], dtype=np.float32))
    im["p1"] = np.ascontiguousarray(np.asarray(inputs["p"][1, b, :S], dtype=np.float32))
    return im


FM1_KINDS = ["cx"] * 4 + ["cz"] * 4 + ["dq"] * 4 + ["dk"] * 4 + ["do"] * 4 + ["dz"] * 4
TM1_COLS = 520


def _perm_rec_w_in(w):
    cols_fm = np.concatenate([np.arange(0, 512), np.arange(512, 1024), np.arange(1024, 1536), np.arange(1536, 2048),
                              np.arange(2568, 3080), np.arange(3080, 3592)])
    cols_tm = np.concatenate([np.arange(2048, 2560), np.arange(2560, 2568)])
    return np.ascontiguousarray(w[:, cols_fm]), np.ascontiguousarray(w[:, cols_tm])


def layer1_consts():
    s = np.arange(64)
    tri = (s[:, None] <= s[None, :]).astype(np.float32)
    e63 = np.zeros((64, 128), np.float32)
    e63[63, :] = 1.0
    return {"tri": tri, "e63": e63}


def rms_to_hnT(c, t, k, src_rows, xt, b_xt, sq, b_sq, st, b_st, gbc, b_g, hn, b_hn, hnT, b_hnT):
    sc = c.sc
    sc.dma("sp", xt[k][:], src_rows, writes=[b_xt[k]])
    sc.dve([b_xt[k]], [b_sq, b_st[k]], "scalar_tensor_tensor", out=sq[:], in0=xt[k][:], scalar=1.0 / D,
           in1=xt[k][:], op0=ALU.mult, op1=ALU.mult, accum_out=st[k][:, 0:1])
    sc.act([b_st[k], c.b_const], [b_st[k]], "activation", out=st[k][:, 1:2], in_=st[k][:, 0:1], func=AF.Sqrt,
           bias=c.eps_t[:, 0:1], scale=1.0)
    sc.dve([b_st[k]], [b_st[k]], "reciprocal", out=st[k][:, 2:3], in_=st[k][:, 1:2])
    sc.dve([b_xt[k], b_st[k], b_g], [b_hn[k]], "scalar_tensor_tensor", out=hn[k][:], in0=xt[k][:],
           scalar=st[k][:, 2:3], in1=gbc[:], op0=ALU.mult, op1=ALU.mult)
    pbk = 6 + k
    tp = bank(c, pbk, 1, BF16)
    for kc in range(8):
        sc.pe([b_hn[k], c.b_const], [c.pb[pbk]], "transpose", out=tp[:, kc * 128:(kc + 1) * 128],
              in_=hn[k][:, kc * 128:(kc + 1) * 128], identity=c.ident_b[:])
    sc.act([c.pb[pbk]], [b_hnT[t]], "activation", out=hnT[:, :, t * 128:(t + 1) * 128],
           in_=tp.rearrange("p (c n) -> p c n", c=8), func=AF.Copy)


def layer1(c, x1_d, out_d):
    nc, sc, S, NT, NG = c.nc, c.sc, c.S, c.NT, c.NG
    NCH = S // 64
    pb = c.pb
    toks = []
    din = c.din
    w1fm_d = din("w1_fm", [D, 3072])
    w1tm_d = din("w1_tm", [D, TM1_COLS])
    g1_d = din("norm_g1", [1, D])
    gf_d = din("final_g", [1, D])
    lruv_d = din("lru_vec", [128, 4, 8])
    wabd_d = din("lru_wa_bd", [4, 128, 128])
    wxbd_d = din("lru_wx_bd", [4, 128, 128])
    mlv_d = din("ml_vec", [128, 8, 5])
    mlb_d = din("ml_b", [1, 8])
    tri_d = din("l1_tri", [64, 64])
    e63_d = din("l1_e63", [64, 128])
    wout_d = din("w_out1", [D, D])
    gw_d = din("ple_gw1", [D, D])
    plew_d = din("ple_w1", [256, D])
    p_d = din("p1", [S, 256])
    U1T_d = c.dscr("U1T_d", [3072, S], F32)

    esL = ExitStack()
    lsb = lambda name, shape, dt=F32: esL.enter_context(nc.sbuf_tensor(name, list(shape), dt))
    V1_d = c.dscr("V1_d", [S, 512], BF16)
    GIF = lsb("GIF", [64, NCH, 8])
    b_V1 = bufs(NCH, "V1")
    b_YcT, b_YdT = Buf("YcT"), bufs(NCH, "YdT")

    with ExitStack() as es:
        tsb = lambda name, shape, dt=F32: es.enter_context(nc.sbuf_tensor(name, list(shape), dt))
        wfm = tsb("w1fm", [128, 8, 3072], BF16)
        wtm = tsb("w1tm", [128, 8, TM1_COLS], BF16)
        hnT = tsb("hnT1", [128, 8, S], BF16)
        gbc = tsb("gbc1", [128, D])
        xt = [tsb(f"xt1{i}", [128, D]) for i in range(2)]
        hn = [tsb(f"hn1{i}", [128, D], BF16) for i in range(2)]
        sq = tsb("sq1", [128, D], BF16)
        st = [tsb(f"st1{i}", [128, 4]) for i in range(2)]
        stage = [tsb(f"stg1{i}", [128, 512]) for i in range(3)]
        b_wfm, b_wtm, b_g = Buf(), Buf(), Buf()
        b_hnT = bufs(NT)
        b_xt, b_hn, b_st, b_sq, b_stage = bufs(2), bufs(2), bufs(2), Buf(), bufs(3)
        for kc in range(8):
            for h in range(2):
                sc.dma("pool", wfm[:, kc, h * 1536:(h + 1) * 1536], w1fm_d[kc * 128:(kc + 1) * 128, h * 1536:(h + 1) * 1536], writes=[b_wfm])
            sc.dma("pool", wtm[:, kc, :], w1tm_d[kc * 128:(kc + 1) * 128, :], writes=[b_wtm])
        sc.dma("sp", gbc[:], g1_d.partition_broadcast(128), writes=[b_g])
        vst = [tsb(f"vst{i}", [64, 512], BF16) for i in range(2)]
        b_vst = bufs(2)
        nst = 0
        nb = 0
        for tg in range(NG):
            for t in range(tg * 4, tg * 4 + 4):
                rms_to_hnT(c, t, t % 2, x1_d[t * 128:(t + 1) * 128, :], xt, b_xt, sq, b_sq, st, b_st, gbc, b_g, hn, b_hn, hnT, b_hnT)
            for ci in range(tg * 8, tg * 8 + 8):
                t = ci // 2
                tcols = slice(ci * 64, ci * 64 + 64)
                k = ci % 2
                psv = bank(c, 4 + k)[0:64, :]
                psg = bank(c, 6 + k)[0:64, 0:8]
                for kc in range(8):
                    sc.pe([b_hnT[t], b_wtm], [pb[4 + k]], "matmul", psv, lhsT=hnT[:, kc, tcols], rhs=wtm[:, kc, 0:512], start=(kc == 0), stop=(kc == 7))
                for kc in range(8):
                    sc.pe([b_hnT[t], b_wtm], [pb[6 + k]], "matmul", psg, lhsT=hnT[:, kc, tcols], rhs=wtm[:, kc, 512:520], start=(kc == 0), stop=(kc == 7))
                sc.act([pb[4 + k]], [b_vst[k]], "activation", out=vst[k][:], in_=psv, func=AF.Copy)
                sc.dma("sp", V1_d[tcols, :], vst[k][:], reads=[b_vst[k]])
                sc.dve([pb[6 + k]], [b_V1[ci]], "tensor_copy", out=GIF[:, ci, :], in_=psg)
            for j, kind in enumerate(FM1_KINDS):
                pbk = nb % 4
                nb += 1
                ps = bank(c, pbk)
                for kc in range(8):
                    sc.pe([b_wfm] + b_hnT[tg * 4:tg * 4 + 4], [pb[pbk]], "matmul", ps, lhsT=wfm[:, kc, j * 128:(j + 1) * 128],
                          rhs=hnT[:, kc, tg * 512:(tg + 1) * 512], start=(kc == 0), stop=(kc == 7))
                s = nst % 3
                nst += 1
                if kind in ("cz", "dz"):
                    sc.act([pb[pbk]], [b_stage[s]], "activation", out=stage[s][:], in_=ps, func=AF.Silu)
                elif kind == "do":
                    sc.act([pb[pbk]], [b_stage[s]], "activation", out=stage[s][:], in_=ps, func=AF.Sigmoid)
                else:
                    sc.dve([pb[pbk]], [b_stage[s]], "tensor_copy", out=stage[s][:], in_=ps)
                sc.dma("sp", U1T_d[j * 128:(j + 1) * 128, tg * 512:(tg + 1) * 512], stage[s][:], reads=[b_stage[s]])
    sc.fence()
    YcT = lsb("YcT", [128, 4, S], BF16)
    YdT = lsb("YdT", [128, 4, S], BF16)

    with ExitStack() as es:
        tsb = lambda name, shape, dt=F32: es.enter_context(nc.sbuf_tensor(name, list(shape), dt))
        lv = tsb("lv", [128, 4, 8])
        c8 = tsb("c8", [128, 4, 4])
        wabd = tsb("wabd", [128, 4, 128], BF16)
        wxbd = tsb("wxbd", [128, 4, 128], BF16)
        b_lv = Buf()
        sc.dma("sp", lv[:], lruv_d, writes=[b_lv])
        for m in range(4):
            sc.dma("pool", wabd[:, m, :], wabd_d[m], writes=[b_lv])
            sc.dma("pool", wxbd[:, m, :], wxbd_d[m], writes=[b_lv])
        sc.act([b_lv], [b_lv], "activation", out=c8[:, :, 0], in_=lv[:, :, 7], func=AF.Exp, scale=-1.0)
        sc.act([b_lv], [b_lv], "activation", out=c8[:, :, 1], in_=c8[:, :, 0], func=AF.Ln, bias=1.0, scale=1.0)
        sc.dve([b_lv], [b_lv], "tensor_scalar", out=c8[:, :, 2], in0=c8[:, :, 1], scalar1=-8.0, scalar2=None, op0=ALU.mult)
        sc.dve([b_lv], [b_lv], "tensor_scalar", out=c8[:, :, 3], in0=c8[:, :, 1], scalar1=-16.0, scalar2=None, op0=ALU.mult)
        cx = tsb("cx", [128, S]); cz = tsb("cz", [128, S]); xc = tsb("xc", [128, S]); Rr = tsb("Rr", [128, S])
        IG = tsb("IG", [128, S]); A2 = tsb("A2", [128, S]); H = tsb("H", [128, S]); xcb = tsb("xcb", [128, S], BF16)
        b_cx, b_cz, b_xc, b_R, b_IG, b_A2, b_H, b_xcb = (Buf() for _ in range(8))
        for m in range(4):
            sc.dma("sp", cx[:], U1T_d[m * 128:(m + 1) * 128, :], writes=[b_cx])
            sc.dma("sp", cz[:], U1T_d[512 + m * 128:512 + (m + 1) * 128, :], writes=[b_cz])
            sc.dve([b_cx, b_lv], [b_xc], "tensor_scalar", out=xc[:], in0=cx[:], scalar1=lv[:, m, 3:4], scalar2=lv[:, m, 4:5], op0=ALU.mult, op1=ALU.add)
            for sh in (1, 2, 3):
                sc.dve([b_cx, b_lv, b_xc], [b_xc], "scalar_tensor_tensor", out=xc[:, sh:S], in0=cx[:, 0:S - sh], scalar=lv[:, m, 3 - sh:4 - sh],
                       in1=xc[:, sh:S], op0=ALU.mult, op1=ALU.add)
            sc.act([b_xc], [b_xcb], "activation", out=xcb[:], in_=xc[:], func=AF.Copy)
            for (wbd, dst, b_dst, bcol) in ((wabd, Rr, b_R, 5), (wxbd, IG, b_IG, 6)):
                for t0 in range(0, S, 2048):
                    tn = min(2048, S - t0)
                    nbk = tn // 512
                    for q in range(nbk):
                        sc.pe([b_xcb, b_lv], [pb[q]], "matmul", bank(c, q), lhsT=wbd[:, m, :], rhs=xcb[:, t0 + q * 512:t0 + (q + 1) * 512], start=True, stop=True)
                    sc.act([pb[q] for q in range(nbk)] + [b_lv], [b_dst], "activation", out=dst[:, t0:t0 + tn], in_=bank(c, 0, nbk), func=AF.Sigmoid,
                           bias=lv[:, m, bcol:bcol + 1], scale=1.0)
            sc.act([b_R, b_lv], [b_A2], "activation", out=A2[:], in_=Rr[:], func=AF.Exp, scale=c8[:, m, 3:4])
            sc.act([b_R, b_lv], [b_R], "activation", out=Rr[:], in_=Rr[:], func=AF.Exp, scale=c8[:, m, 2:3])
            sc.dve([b_A2], [b_A2], "tensor_scalar", out=A2[:], in0=A2[:], scalar1=-1.0, scalar2=1.0, op0=ALU.mult, op1=ALU.add)
            sc.act([b_A2], [b_A2], "activation", out=A2[:], in_=A2[:], func=AF.Sqrt)
            sc.dve([b_IG, b_xc], [b_IG], "tensor_tensor", out=IG[:], in0=IG[:], in1=xc[:], op=ALU.mult)
            sc.dve([b_IG, b_A2], [b_IG], "tensor_tensor", out=IG[:], in0=IG[:], in1=A2[:], op=ALU.mult)
            sc.dve([b_R, b_IG], [b_H], "tensor_tensor_scan", out=H[:], data0=Rr[:], data1=IG[:], initial=0.0, op0=ALU.mult, op1=ALU.add)
            sc.dve([b_H, b_cz], [b_YcT], "tensor_tensor", out=YcT[:, m, :], in0=H[:], in1=cz[:], op=ALU.mult)
    sc.fence()

    with ExitStack() as es:
        tsb = lambda name, shape, dt=F32: es.enter_context(nc.sbuf_tensor(name, list(shape), dt))
        QK = tsb("QK", [128, 8, S], BF16)
        mlv = tsb("mlv", [128, 8, 5])
        mlb = tsb("mlb", [64, 8])
        tri = tsb("tri", [64, 64])
        e63 = tsb("e63", [64, 128])
        b_ml = Buf()
        sc.dma("sp", mlv[:], mlv_d, writes=[b_ml])
        sc.dma("sp", mlb[:], mlb_d.partition_broadcast(64), writes=[b_ml])
        sc.dma("sp", tri[:], tri_d, writes=[b_ml])
        sc.dma("sp", e63[:], e63_d, writes=[b_ml])
        b_QK = Buf()
        with ExitStack() as es2:
            tsb2 = lambda name, shape, dt=F32: es2.enter_context(nc.sbuf_tensor(name, list(shape), dt))
            cu = tsb2("cu", [128, S]); xq = tsb2("xq", [128, S])
            b_cu, b_xq = Buf(), Buf()
            for j in range(8):
                sc.dma("sp", cu[:], U1T_d[1024 + j * 128:1024 + (j + 1) * 128, :], writes=[b_cu])
                sc.dve([b_cu, b_ml], [b_xq], "tensor_scalar", out=xq[:], in0=cu[:], scalar1=mlv[:, j, 3:4], scalar2=None, op0=ALU.mult)
                for sh in (1, 2, 3):
                    sc.dve([b_cu, b_ml, b_xq], [b_xq], "scalar_tensor_tensor", out=xq[:, sh:S], in0=cu[:, 0:S - sh], scalar=mlv[:, j, 3 - sh:4 - sh],
                           in1=xq[:, sh:S], op0=ALU.mult, op1=ALU.add)
                if j < 4:
                    sc.act([b_xq, b_ml], [b_QK], "activation", out=QK[:, j, :], in_=xq[:], func=AF.Silu, bias=mlv[:, j, 4:5], scale=1.0)
                else:
                    sc.act([b_xq, b_ml], [b_xq], "activation", out=xq[:], in_=xq[:], func=AF.Silu, bias=mlv[:, j, 4:5], scale=1.0)
                    sc.dve([b_xq], [b_QK], "tensor_scalar", out=QK[:, j, :], in0=xq[:], scalar1=128.0 ** -0.5, scalar2=None, op0=ALU.mult)
        sc.fence()
        LF = tsb("LF", [64, NCH, 4]); FL = tsb("FL", [64, NCH, 4]); Am = tsb("Am", [64, NCH, 4]); Bm = tsb("Bm", [64, NCH, 4])
        AE = tsb("AE", [128, NCH, 4])
        b_gt = Buf()
        sc.dve(b_V1 + [b_ml], [b_gt], "tensor_tensor", out=GIF[:], in0=GIF[:], in1=mlb[:].unsqueeze(1).to_broadcast([64, NCH, 8]), op=ALU.add)
        sc.act([b_gt], [b_gt], "activation", out=LF[:], in_=GIF[:, :, 4:8], func=AF.Exp, scale=-1.0)
        sc.act([b_gt], [b_gt], "activation", out=LF[:], in_=LF[:], func=AF.Ln, bias=1.0, scale=1.0)
        sc.dve([b_gt], [b_gt], "tensor_scalar", out=LF[:], in0=LF[:], scalar1=-1.0, scalar2=None, op0=ALU.mult)
        ng = NCH * 4
        for n0 in range(0, ng, 512):
            nn = min(512, ng - n0)
            sc.pe([b_gt, b_ml], [pb[0]], "matmul", bank(c, 0)[0:64, 0:nn], lhsT=tri[:], rhs=LF[:].rearrange("p c h -> p (c h)")[:, n0:n0 + nn], start=True, stop=True)
            sc.dve([pb[0]], [b_gt], "tensor_copy", out=FL[:].rearrange("p c h -> p (c h)")[:, n0:n0 + nn], in_=bank(c, 0)[0:64, 0:nn])
        sc.act([b_gt], [b_gt], "activation", out=Am[:], in_=FL[:], func=AF.Exp)
        sc.dve([b_gt], [b_gt], "tensor_tensor", out=Bm[:], in0=GIF[:, :, 0:4], in1=FL[:], op=ALU.subtract)
        sc.act([b_gt], [b_gt], "activation", out=Bm[:], in_=Bm[:], func=AF.Exp)
        for n0 in range(0, ng, 512):
            nn = min(512, ng - n0)
            sc.pe([b_gt, b_ml], [pb[1]], "matmul", bank(c, 1)[:, 0:nn], lhsT=e63[:], rhs=Am[:].rearrange("p c h -> p (c h)")[:, n0:n0 + nn], start=True, stop=True)
            sc.dve([pb[1]], [b_gt], "tensor_copy", out=AE[:].rearrange("p c h -> p (c h)")[:, n0:n0 + nn], in_=bank(c, 1)[:, 0:nn])

        Cst = tsb("Cst", [128, 4, 129]); Cb = tsb("Cb", [128, 4, 129], BF16)
        b_C, b_Cb = bufs(4), bufs(4)
        sc.dve([], b_C, "memset", Cst[:], 0.0)
        sc.pool([], b_Cb, "memset", Cb[:], 0.0)
        Wp = [tsb(f"Wp{i}", [64, 64], BF16) for i in range(2)]
        kB = [tsb(f"kB{i}", [64, 128], BF16) for i in range(2)]
        XY = [tsb(f"XY{i}", [64, 4, 129]) for i in range(2)]
        b_Wp, b_kB, b_XY = bufs(2), bufs(2), bufs(2)
        fin = tsb("fin", [64, 4, 4]); HD = tsb("HD", [64, 4, 128], BF16)
        Vc = [tsb(f"Vc{i}", [64, 4, 129], BF16) for i in range(2)]
        b_Vc = bufs(2)
        for i in range(2):
            sc.pool([], [b_Vc[i]], "memset", Vc[i][:, :, 128:129], 1.0)
        oz2 = [tsb(f"oz{i}", [128, 2, 4, 64]) for i in range(2)]
        b_oz2 = bufs(2)
        b_fin, b_HD = Buf(), Buf()
        sc.fence()
        Wp4 = [tsb(f"Wp4_{h}", [64, 64], BF16) for h in range(4)]
        kB4 = [tsb(f"kB4_{h}", [64, 128], BF16) for h in range(4)]
        b_Wp4, b_kB4 = bufs(4), bufs(4)
        b_ps_s, b_ps_x, b_ps_k, b_ps_u = [pb[0]] * 4, [pb[1], pb[1], pb[2], pb[2]], [pb[3]] * 4, [pb[4], pb[4], pb[5], pb[5]]
        b_XY4 = [bufs(4), bufs(4)]
        def ml_F1(ci):
            ksl = slice(ci * 64, ci * 64 + 64)
            xk = ci % 2
            oz, b_oz = oz2[xk], b_oz2[xk]
            ps_s = [bank(c, 0)[0:64, h * 64:(h + 1) * 64] for h in range(4)]
            ps_x = [bank(c, 1 + h // 2)[0:64, (h % 2) * 129:(h % 2) * 129 + 129] for h in range(4)]
            ps_k = [bank(c, 3, 1, BF16)[0:64, h * 128:(h + 1) * 128] for h in range(4)]
            ps_u = [bank(c, 4 + h // 2)[:, (h % 2) * 129:(h % 2) * 129 + 129] for h in range(4)]
            sc.dma("sp", Vc[xk][:, :, 0:128], V1_d[ksl, :].rearrange("p (h d) -> p h d", h=4), writes=[b_Vc[xk]])
            sc.dma("sp", oz[:, 0, :, :], U1T_d.rearrange("(j p) s -> p j s", p=128)[:, 16:20, ksl], writes=[b_oz])
            sc.dma("sp", oz[:, 1, :, :], U1T_d.rearrange("(j p) s -> p j s", p=128)[:, 20:24, ksl], writes=[b_oz])
            sc.pool([b_oz], [b_oz], "tensor_tensor", out=oz[:, 0, :, :], in0=oz[:, 0, :, :], in1=oz[:, 1, :, :], op=ALU.mult)
            for h in range(4):
                sc.pe([b_QK], [b_ps_s[h]], "matmul", ps_s[h], lhsT=QK[:, 4 + h, ksl], rhs=QK[:, h, ksl], start=True, stop=True)
            for h in range(4):
                sc.pe([b_QK, c.b_const], [b_ps_k[h]], "transpose", out=ps_k[h], in_=QK[:, 4 + h, ksl], identity=c.ident_b[:])
            for h in range(4):
                sc.dve([b_ps_s[h], b_gt, b_ml], [b_Wp4[h]], "scalar_tensor_tensor", out=Wp4[h][:], in0=ps_s[h], scalar=Bm[:, ci, h:h + 1], in1=tri[:],
                       op0=ALU.mult, op1=ALU.mult)
            for h in range(4):
                sc.dve([b_ps_k[h], b_gt], [b_kB4[h]], "tensor_scalar", out=kB4[h][:], in0=ps_k[h], scalar1=Bm[:, ci, h:h + 1], scalar2=AE[0:64, ci, h:h + 1],
                       op0=ALU.mult, op1=ALU.mult)

        def ml_F2(ci):
            ksl = slice(ci * 64, ci * 64 + 64)
            xk = ci % 2
            oz, b_oz = oz2[xk], b_oz2[xk]
            ps_s = [bank(c, 0)[0:64, h * 64:(h + 1) * 64] for h in range(4)]
            ps_x = [bank(c, 1 + h // 2)[0:64, (h % 2) * 129:(h % 2) * 129 + 129] for h in range(4)]
            ps_k = [bank(c, 3, 1, BF16)[0:64, h * 128:(h + 1) * 128] for h in range(4)]
            ps_u = [bank(c, 4 + h // 2)[:, (h % 2) * 129:(h % 2) * 129 + 129] for h in range(4)]
            for h in range(4):
                sc.pe([b_Wp4[h], b_Vc[xk]], [b_ps_x[h]], "matmul", ps_x[h], lhsT=Wp4[h][:], rhs=Vc[xk][:, h, :], start=True, stop=False)
                sc.pe([b_QK, b_Cb[h]], [b_ps_x[h]], "matmul", ps_x[h], lhsT=QK[:, h, ksl], rhs=Cb[:, h, :], start=False, stop=True)
            for h in range(4):
                sc.act([b_ps_x[h]], [b_XY4[xk][h]], "activation", out=XY[xk][:, h, :], in_=ps_x[h], func=AF.Copy)
            for h in range(4):
                sc.pe([b_kB4[h], b_Vc[xk]], [b_ps_u[h]], "matmul", ps_u[h], lhsT=kB4[h][:], rhs=Vc[xk][:, h, :], start=True, stop=True)

        def ml_F3(ci):
            ksl = slice(ci * 64, ci * 64 + 64)
            xk = ci % 2
            oz, b_oz = oz2[xk], b_oz2[xk]
            ps_s = [bank(c, 0)[0:64, h * 64:(h + 1) * 64] for h in range(4)]
            ps_x = [bank(c, 1 + h // 2)[0:64, (h % 2) * 129:(h % 2) * 129 + 129] for h in range(4)]
            ps_k = [bank(c, 3, 1, BF16)[0:64, h * 128:(h + 1) * 128] for h in range(4)]
            ps_u = [bank(c, 4 + h // 2)[:, (h % 2) * 129:(h % 2) * 129 + 129] for h in range(4)]
            for h in range(4):
                sc.dve([b_C[h], b_ps_u[h], b_gt], [b_C[h]], "scalar_tensor_tensor", out=Cst[:, h, :], in0=Cst[:, h, :], scalar=AE[:, ci, h:h + 1], in1=ps_u[h],
                       op0=ALU.mult, op1=ALU.add)
            for h in range(4):
                sc.act([b_C[h]], [b_Cb[h]], "activation", out=Cb[:, h, :], in_=Cst[:, h, :], func=AF.Copy)

        def ml_Ba(ci):
            ksl = slice(ci * 64, ci * 64 + 64)
            xk = ci % 2
            oz, b_oz = oz2[xk], b_oz2[xk]
            ps_s = [bank(c, 0)[0:64, h * 64:(h + 1) * 64] for h in range(4)]
            ps_x = [bank(c, 1 + h // 2)[0:64, (h % 2) * 129:(h % 2) * 129 + 129] for h in range(4)]
            ps_k = [bank(c, 3, 1, BF16)[0:64, h * 128:(h + 1) * 128] for h in range(4)]
            ps_u = [bank(c, 4 + h // 2)[:, (h % 2) * 129:(h % 2) * 129 + 129] for h in range(4)]
            X = XY[xk]
            sc.dve(b_XY4[xk] + [b_gt], [b_fin], "tensor_tensor", out=fin[:, :, 0], in0=X[:, :, 128], in1=Am[:, ci, :], op=ALU.mult)
            sc.dve([b_fin], [b_fin], "tensor_scalar", out=fin[:, :, 1], in0=fin[:, :, 0], scalar1=-1.0, scalar2=None, op0=ALU.mult)
            sc.dve([b_fin], [b_fin], "tensor_tensor", out=fin[:, :, 1], in0=fin[:, :, 1], in1=fin[:, :, 0], op=ALU.max)
            sc.dve([b_fin], [b_fin], "tensor_scalar", out=fin[:, :, 1], in0=fin[:, :, 1], scalar1=1.0, scalar2=None, op0=ALU.max)
            sc.dve([b_fin], [b_fin], "reciprocal", out=fin[:, :, 2], in_=fin[:, :, 1])
            sc.dve([b_fin, b_gt], [b_fin], "tensor_tensor", out=fin[:, :, 3], in0=fin[:, :, 2], in1=Am[:, ci, :], op=ALU.mult)
            sc.dve(b_XY4[xk] + [b_fin], [b_HD], "tensor_tensor", out=HD[:], in0=X[:, :, 0:128], in1=fin[:, :, 3:4].to_broadcast([64, 4, 128]), op=ALU.mult)

        def ml_Bb(ci):
            ksl = slice(ci * 64, ci * 64 + 64)
            xk = ci % 2
            oz, b_oz = oz2[xk], b_oz2[xk]
            ps_s = [bank(c, 0)[0:64, h * 64:(h + 1) * 64] for h in range(4)]
            ps_x = [bank(c, 1 + h // 2)[0:64, (h % 2) * 129:(h % 2) * 129 + 129] for h in range(4)]
            ps_k = [bank(c, 3, 1, BF16)[0:64, h * 128:(h + 1) * 128] for h in range(4)]
            ps_u = [bank(c, 4 + h // 2)[:, (h % 2) * 129:(h % 2) * 129 + 129] for h in range(4)]
            tpb = 6 + (ci % 2)
            tph = bank(c, tpb, 1, BF16)[:, 0:256].rearrange("p (h l) -> p h l", h=4)
            for h in range(4):
                sc.pe([b_HD, c.b_const], [pb[tpb]], "transpose", out=tph[:, h, :], in_=HD[:, h, :], identity=c.ident_b[0:64, 0:64])
            sc.dve([pb[tpb], b_oz], [b_YdT[ci]], "tensor_tensor", out=YdT[:, :, ksl], in0=tph, in1=oz[:, 0, :, :], op=ALU.mult)

        ml_F1(0)
        ml_F2(0)
        ml_F3(0)
        for ci in range(NCH):
            nxt = ci + 1 < NCH
            if nxt:
                ml_F1(ci + 1)
            ml_Ba(ci)
            if nxt:
                ml_F2(ci + 1)
            ml_Bb(ci)
            if nxt:
                ml_F3(ci + 1)
    sc.fence()

    with ExitStack() as es:
        tsb = lambda name, shape, dt=F32: es.enter_context(nc.sbuf_tensor(name, list(shape), dt))
        wout = tsb("wout1", [128, 8, D], BF16)
        gw = tsb("gw1", [128, 8, D], BF16)
        plew = tsb("plew1", [128, 2, D], BF16)
        gfb = tsb("gfb", [128, D])
        b_w = Buf()
        for kc in range(8):
            sc.dma("pool", wout[:, kc, :], wout_d[kc * 128:(kc + 1) * 128, :], writes=[b_w])
            sc.dma("pool", gw[:, kc, :], gw_d[kc * 128:(kc + 1) * 128, :], writes=[b_w])
        for kc in range(2):
            sc.dma("pool", plew[:, kc, :], plew_d[kc * 128:(kc + 1) * 128, :], writes=[b_w])
        sc.dma("sp", gfb[:], gf_d.partition_broadcast(128), writes=[b_w])
        bf = {}
        for nm, shape, dt in (("x_t", [128, D], F32), ("p_t", [128, 256], F32), ("XA", [128, D], F32), ("XAb", [128, D], BF16),
                              ("XAT", [128, 8, 128], BF16), ("Pb", [128, 256], BF16), ("PTT", [128, 2, 128], BF16), ("SG", [128, D], F32),
                              ("XB", [128, D], F32), ("sq", [128, D], BF16), ("st", [128, 4], F32), ("OUT", [128, D], F32)):
            bf[nm] = (tsb("l1" + nm, shape, dt), Buf())
        for i in range(NT):
            cols = slice(i * 128, (i + 1) * 128)
            x_t, b_x = bf["x_t"]; p_t, b_p = bf["p_t"]
            sc.dma("sp", x_t[:], x1_d[cols, :], writes=[b_x])
            sc.dma("sp", p_t[:], p_d[cols, :], writes=[b_p])
            for half in range(2):
                for kc in range(8):
                    Y = YcT if kc < 4 else YdT
                    rd = [b_YcT] if kc < 4 else [b_YdT[2 * i], b_YdT[2 * i + 1]]
                    sc.pe(rd + [b_w], [pb[6 + half]], "matmul", bank(c, 6 + half), lhsT=Y[:, kc % 4, cols], rhs=wout[:, kc, half * 512:(half + 1) * 512],
                          start=(kc == 0), stop=(kc == 7))
            toks.append(tail_tile(c, bf, gw, plew, b_w, out_d[cols, :], gfb))
    esL.close()
    sc.fence()
    return toks


def tail_tile(c, bf, gw, plew, b_w, dst, gfb):
    sc, pb = c.sc, c.pb
    x_t, b_x = bf["x_t"]; p_t, b_p = bf["p_t"]; XA, b_XA = bf["XA"]; XAb, b_XAb = bf["XAb"]; XAT, b_XAT = bf["XAT"]
    Pb, b_Pb = bf["Pb"]; PTT, b_PTT = bf["PTT"]; SG, b_SG = bf["SG"]; XB, b_XB = bf["XB"]
    sc.dve([b_x, pb[6], pb[7]], [b_XA], "tensor_tensor", out=XA[:], in0=x_t[:], in1=bank(c, 6, 2), op=ALU.add)
    sc.act([b_XA], [b_XAb], "activation", out=XAb[:], in_=XA[:], func=AF.Copy)
    sc.act([b_p], [b_Pb], "activation", out=Pb[:], in_=p_t[:], func=AF.Copy)
    tp = bank(c, 6, 1, BF16)
    for kc in range(8):
        sc.pe([b_XAb, c.b_const], [pb[6]], "transpose", out=tp[:, kc * 128:(kc + 1) * 128], in_=XAb[:, kc * 128:(kc + 1) * 128], identity=c.ident_b[:])
    sc.dve([pb[6]], [b_XAT], "tensor_copy", out=XAT[:], in_=tp.rearrange("p (c n) -> p c n", c=8))
    tp2 = bank(c, 7, 1, BF16)
    for kc in range(2):
        sc.pe([b_Pb, c.b_const], [pb[7]], "transpose", out=tp2[:, kc * 128:(kc + 1) * 128], in_=Pb[:, kc * 128:(kc + 1) * 128], identity=c.ident_b[:])
    sc.dve([pb[7]], [b_PTT], "tensor_copy", out=PTT[:], in_=tp2[:, 0:256].rearrange("p (c n) -> p c n", c=2))
    for half in range(2):
        for kc in range(8):
            sc.pe([b_XAT, b_w], [pb[4 + half]], "matmul", bank(c, 4 + half), lhsT=XAT[:, kc, :], rhs=gw[:, kc, half * 512:(half + 1) * 512],
                  start=(kc == 0), stop=(kc == 7))
    sc.act([pb[4], pb[5]], [b_SG], "activation", out=SG[:], in_=bank(c, 4, 2), func=AF.Sigmoid)
    for half in range(2):
        for kc in range(2):
            sc.pe([b_PTT, b_w], [pb[6 + half]], "matmul", bank(c, 6 + half), lhsT=PTT[:, kc, :], rhs=plew[:, kc, half * 512:(half + 1) * 512],
                  start=(kc == 0), stop=(kc == 1))
    sc.dve([b_SG, pb[6], pb[7]], [b_XB], "tensor_tensor", out=XB[:], in0=SG[:], in1=bank(c, 6, 2), op=ALU.mult)
    sc.dve([b_XB, b_XA], [b_XB], "tensor_tensor", out=XB[:], in0=XB[:], in1=XA[:], op=ALU.add)
    if gfb is None:
        return sc.dma("sp", dst, XB[:], reads=[b_XB])
    sq, b_sq = bf["sq"]; st, b_st = bf["st"]; OUT, b_OUT = bf["OUT"]
    sc.dve([b_XB], [b_sq, b_st], "scalar_tensor_tensor", out=sq[:], in0=XB[:], scalar=1.0 / D, in1=XB[:], op0=ALU.mult, op1=ALU.mult,
           accum_out=st[:, 0:1])
    sc.act([b_st, c.b_const], [b_st], "activation", out=st[:, 1:2], in_=st[:, 0:1], func=AF.Sqrt, bias=c.eps_t[:, 0:1], scale=1.0)
    sc.dve([b_st], [b_st], "reciprocal", out=st[:, 2:3], in_=st[:, 1:2])
    sc.dve([b_XB, b_st, b_w], [b_OUT], "scalar_tensor_tensor", out=OUT[:], in0=XB[:], scalar=st[:, 2:3], in1=gfb[:], op0=ALU.mult, op1=ALU.mult)
    return sc.dma("sp", dst, OUT[:], reads=[b_OUT])


_PROGRAM_CACHE = {}


def kernel(**inputs):
    S = 4096
    B = 8
    if S not in _PROGRAM_CACHE:
        _PROGRAM_CACHE[S] = build_program(S, stages=("l0", "l1"))
    nc = _PROGRAM_CACHE[S]
    shared = shared_inputs(inputs, S)
    in_maps = [core_inputs(inputs, b, S, shared) for b in range(B)]
    res = run_bass_kernel_spmd(nc, in_maps, core_ids=list(range(B)))
    out = np.stack([np.asarray(r["out"], dtype=np.float32) for r in res.results], axis=0)
    return out
```

```python
import math
from contextlib import ExitStack

import numpy as np
import ml_dtypes

import concourse.bass as bass
import concourse.mybir as mybir
from concourse.bass_utils import run_bass_kernel_spmd

F32 = mybir.dt.float32
BF16 = mybir.dt.bfloat16
I32 = mybir.dt.int32
AF = mybir.ActivationFunctionType
ALU = mybir.AluOpType
AX = mybir.AxisListType

D = 1024
NEGM = -30000.0


class Buf:
    __slots__ = ("w", "r", "name")

    def __init__(self, name=""):
        self.w = None
        self.r = {}
        self.name = name


def bufs(n, name=""):
    return [Buf(f"{name}{i}") for i in range(n)]


class Sched:
    ENG = ("pe", "act", "dve", "pool", "sp")
    NDMA = 8

    def __init__(self, nc):
        self.nc = nc
        self.eobj = {"pe": nc.tensor, "act": nc.scalar, "dve": nc.vector, "pool": nc.gpsimd, "sp": nc.sync}
        self.sem = {}
        for e in self.ENG:
            self.sem[e] = nc.alloc_semaphore(f"s_{e}")
        self.cnt = {e: 0 for e in self.ENG}
        self.seen = {e: {} for e in self.ENG}
        self.snap = {}
        self.prog = {e: [] for e in self.ENG}
        self.dma_i = {}
        self.dma_val = {}
        for q in ("sp", "pool", "act"):
            for k in range(self.NDMA):
                key = f"d_{q}{k}"
                self.sem[key] = nc.alloc_semaphore(key)
                self.dma_val[key] = 0
            self.dma_i[q] = 0
        self.n_ops = 0
        self.n_waits = 0
        self.limit = None
        self.pending_fence = {e: [] for e in self.ENG}

    def fence(self):
        toks = [(e, self.cnt[e]) for e in self.ENG if self.cnt[e] > 0]
        toks += [(k, v) for k, v in self.dma_val.items() if v > 0]
        for e in self.ENG:
            self.pending_fence[e] = list(toks)

    def _collect(self, eng, reads, writes, extra=()):
        need = {}

        def add(tok):
            if tok is None:
                return
            k, v = tok
            if k == eng and eng == "pe":
                return
            if self.seen[eng].get(k, 0) >= v:
                return
            if need.get(k, 0) < v:
                need[k] = v

        for b in reads:
            add(b.w)
        for b in writes:
            add(b.w)
            for k, v in b.r.items():
                add((k, v))
        for t in extra:
            add(t)
        if self.pending_fence[eng]:
            for t in self.pending_fence[eng]:
                if not (t[0] == eng and eng in ("pe",)) or True:
                    k, v = t
                    if self.seen[eng].get(k, 0) < v and need.get(k, 0) < v and not (k == eng and False):
                        need[k] = v
            self.pending_fence[eng] = []
        sn = self.seen[eng]
        for k, v in need.items():
            sn[k] = max(sn.get(k, 0), v)
            other = self.snap.get((k, v))
            if other:
                for k2, v2 in other.items():
                    if sn.get(k2, 0) < v2:
                        sn[k2] = v2
        return list(need.items())

    def op(self, eng, meth, reads, writes, *a, **kw):
        if self.limit is not None and self.n_ops >= self.limit:
            return None
        waits = self._collect(eng, reads, writes)
        self.cnt[eng] += 1
        me = (eng, self.cnt[eng])
        for b in reads:
            b.r[eng] = me[1]
        for b in writes:
            b.w = me
            b.r = {}
        self.snap[me] = dict(self.seen[eng])
        self.prog[eng].append((waits, (meth, a, kw), self.sem[eng], 1))
        self.n_ops += 1
        self.n_waits += len(waits)
        return me

    def pe(self, reads, writes, meth, *a, **kw):
        return self.op("pe", meth, reads, writes, *a, **kw)

    def act(self, reads, writes, meth, *a, **kw):
        return self.op("act", meth, reads, writes, *a, **kw)

    def dve(self, reads, writes, meth, *a, **kw):
        return self.op("dve", meth, reads, writes, *a, **kw)

    def pool(self, reads, writes, meth, *a, **kw):
        return self.op("pool", meth, reads, writes, *a, **kw)

    def dma(self, q, out, in_, reads=(), writes=(), **kw):
        if self.limit is not None and self.n_ops >= self.limit:
            return None
        i = self.dma_i[q]
        self.dma_i[q] += 1
        key = f"d_{q}{i % self.NDMA}"
        prev = self.dma_val[key]
        extra = [(key, prev)] if prev > 0 else []
        waits = self._collect(q, reads, writes, extra)
        self.dma_val[key] = prev + 16
        me = (key, prev + 16)
        for b in reads:
            b.r[key] = me[1]
        for b in writes:
            b.w = me
            b.r = {}
        kw = dict(kw, out=out, in_=in_)
        self.prog[q].append((waits, ("dma_start", (), kw), self.sem[key], 16))
        self.n_ops += 1
        self.n_waits += len(waits)
        return me

    def wait_all(self, eng, toks):
        waits = []
        for t in toks:
            if t is not None:
                waits.append(t)
        if self.limit is not None:
            waits = [(e, self.cnt[e]) for e in self.ENG if self.cnt[e] > 0]
            waits += [(k, v) for k, v in self.dma_val.items() if v > 0]
        self.prog[eng].append((waits, None, None, 0))

    def emit(self):
        nc = self.nc
        with nc.Block() as block:
            def run(engname):
                def body(e):
                    regcache = {}
                    for waits, fn, sem, inc in self.prog[engname]:
                        for k, v in waits:
                            e.wait_ge(self.sem[k], v)
                        if fn is not None:
                            meth, a, kw = fn
                            if meth == "affine_select":
                                fv = kw["fill"]
                                if fv not in regcache:
                                    regcache[fv] = e.to_reg(fv)
                                kw = dict(kw, fill=regcache[fv])
                            try:
                                ins = getattr(e, meth)(*a, **kw)
                            except Exception:
                                print("EMIT FAIL", engname, meth, [getattr(x, "shape", x) for x in a],
                                      {k: (getattr(v, "shape", v), getattr(v, "ap", None)) for k, v in kw.items()})
                                raise
                            ins.then_inc(sem, inc)
                return body
            block.tensor(run("pe"))
            block.scalar(run("act"))
            block.vector(run("dve"))
            block.gpsimd(run("pool"))
            block.sync(run("sp"))


ATTN_IN = 3292
REC_IN = 3592
RMS_EPS = 1e-6

FM0 = [("qa", 512), ("qb", 512), ("qi", 256), ("kc", 128), ("vc", 128), ("ks", 128), ("kw", 128),
       ("kbki", 128), ("za", 512), ("zb", 512), ("g", 24)]
FM0_COLS = sum(c for _, c in FM0)
TM0_COLS = 324


def _perm_attn_w_in(w):
    o = {}
    o["a_q"] = 0
    o["a_kv"] = 512
    o["a_g"] = 1280
    o["a_z"] = 1304
    o["b_q"] = 1816
    o["b_k"] = 2328
    o["b_v"] = 2392
    o["b_qi"] = 2456
    o["b_ki"] = 2712
    o["b_wi"] = 2776
    o["b_z"] = 2780
    qa = []
    for m in range(4):
        for g in range(2):
            st = o["a_q"] + g * 256 + m * 64
            qa.append(np.arange(st, st + 64))
    qa = np.concatenate(qa)
    kv = o["a_kv"]
    cols_fm = np.concatenate([
        qa,
        np.arange(o["b_q"], o["b_q"] + 512),
        np.arange(o["b_qi"], o["b_qi"] + 256),
        np.arange(kv + 0, kv + 128),
        np.arange(kv + 128, kv + 256),
        np.arange(kv + 256, kv + 384),
        np.arange(kv + 512, kv + 640),
        np.arange(o["b_k"], o["b_k"] + 64), np.arange(o["b_ki"], o["b_ki"] + 64),
        np.arange(o["a_z"], o["a_z"] + 512),
        np.arange(o["b_z"], o["b_z"] + 512),
        np.arange(o["a_g"], o["a_g"] + 24),
    ])
    cols_tm = np.concatenate([
        np.arange(kv + 384, kv + 512),
        np.arange(kv + 640, kv + 768),
        np.arange(o["b_v"], o["b_v"] + 64),
        np.arange(o["b_wi"], o["b_wi"] + 4),
    ])
    assert len(cols_fm) == FM0_COLS and len(cols_tm) == TM0_COLS
    return np.ascontiguousarray(w[:, cols_fm]), np.ascontiguousarray(w[:, cols_tm])


class Ctx:
    pass


def bank(c, i, n=1, dt=F32):
    ap = c.psum[:, i * 512:(i + n) * 512]
    if dt != F32:
        ap = ap.bitcast(dt)
    return ap


def build_program(S, stages=("l0proj",), dbg=(), limit=None):
    nc = bass.Bass("TRN2", target_bir_lowering=False)
    NT = S // 128
    NG = S // 512
    sc = Sched(nc)
    sc.limit = limit
    es = ExitStack()
    c = Ctx()
    c.nc, c.sc, c.S, c.NT, c.NG = nc, sc, S, NT, NG
    c.dbg = dbg
    c.dbg_toks = []

    def din(name, shape, dt=F32):
        return nc.dram_tensor(name, list(shape), dt, kind="ExternalInput").ap()

    def dscr(name, shape, dt=F32):
        if name in dbg:
            return nc.dram_tensor(name, list(shape), dt, kind="ExternalOutput").ap()
        return nc.dram_tensor(name, list(shape), dt).ap()

    def dout(name, shape, dt=F32):
        return nc.dram_tensor(name, list(shape), dt, kind="ExternalOutput").ap()

    def sb(name, shape, dt=F32):
        return es.enter_context(nc.sbuf_tensor(name, list(shape), dt))

    def dump(name, ap, reads):
        if name not in dbg or sc.limit is not None:
            return
        o = dout("dbg_" + name, list(ap.shape), ap.dtype)
        c.dbg_toks.append(sc.dma("sp", o, ap, reads=reads))

    c.din, c.dscr, c.dout, c.sb, c.dump = din, dscr, dout, sb, dump

    c.x = din("x", [S, D])
    c.norm_g0 = din("norm_g0", [1, D])
    c.w0_fm = din("w0_fm", [D, FM0_COLS])
    c.w0_tm = din("w0_tm", [D, TM0_COLS])

    c.psum = es.enter_context(nc.psum_tensor("ps", [128, 4096], F32))
    c.pb = bufs(8, "psb")

    c.ident_f = sb("ident_f", [128, 128], F32)
    c.ident_b = sb("ident_b", [128, 128], BF16)
    c.eps_t = sb("eps_t", [128, 1], F32)
    c.b_const = Buf("const")
    idn = din("ident", [128, 128])
    sc.dma("sp", c.ident_f[:], idn, writes=[c.b_const])
    sc.dve([c.b_const], [c.b_const], "tensor_copy", out=c.ident_b[:], in_=c.ident_f[:])
    sc.dve([], [c.b_const], "memset", c.eps_t[:], RMS_EPS)

    finals = []
    c.hc = {}
    x1_d = dscr("x1_d", [S, D])
    if "l0" in stages:
        NB = S // 64
        for name, shape in (("Fc", [S, NB]), ("Bc", [128, 8, 16]), ("AB0", [128, 8, 128]), ("AB1", [128, 8, 128]),
                            ("DB0", [128, 8, 128]), ("DB1", [128, 8, 128]), ("W4", [128, 4, 128]), ("tb31", [1, 16]),
                            ("I4", [128, 512])):
            c.hc[name] = din("hc_" + name, shape)
        es0 = ExitStack()
        c.es = es0
        layer0_alloc(c)
        esA = ExitStack()
        c.KcT = esA.enter_context(nc.sbuf_tensor("KcT", [128, S + 32], BF16))
        c.VcT = esA.enter_context(nc.sbuf_tensor("VcT", [128, S + 32], BF16))
        finals += layer0_proj(c)
        sc.fence()
        layer0_cmp(c)
        esA.close()
        sc.fence()
        finals += layer0_attn(c, x1_d)
        es0.close()
        sc.fence()

    if "l1" in stages:
        out_d = dout("out", [S, D])
        finals += layer1(c, x1_d, out_d)

    sc.wait_all("sp", finals + c.dbg_toks)
    sc.emit()
    es.close()
    return nc


def layer0_alloc(c):
    S, NT, NG = c.S, c.NT, c.NG
    sb = lambda name, shape, dt=F32: c.es.enter_context(c.nc.sbuf_tensor(name, list(shape), dt))
    c.KsT = sb("KsT", [128, S], BF16)
    c.KwT = sb("KwT", [128, S], BF16)
    c.KbKi = sb("KbKi", [128, S], BF16)
    c.Vs = sb("Vs", [128, NT, 2, 65], BF16)
    c.Vw = sb("Vw", [128, NT, 2, 65], BF16)
    c.Vb = sb("Vb", [128, NT, 65], BF16)
    c.WI = sb("WI", [128, NT, 4], F32)
    n_cmp = S // 16 - 1
    c.KcmpT = sb("KcmpT", [128, S // 16], BF16)
    c.Vcmp = sb("Vcmp", [128, (n_cmp + 127) // 128, 2, 64], BF16)
    c.b_KcT, c.b_VcT, c.b_KsT, c.b_KwT, c.b_KbKi = (bufs(NG, n) for n in ("KcT", "VcT", "KsT", "KwT", "KbKi"))
    c.b_V = bufs(NT, "V")


def layer0_proj(c):
    nc, sc, S, NT, NG = c.nc, c.sc, c.S, c.NT, c.NG
    sb, dscr = c.sb, c.dscr
    toks = []
    c.QaT_d = dscr("QaT_d", [512, S], BF16)
    c.QbT_d = dscr("QbT_d", [512, S], BF16)
    c.QiT_d = dscr("QiT_d", [256, S], BF16)
    c.ZaT_d = dscr("ZaT_d", [512, S], BF16)
    c.ZbT_d = dscr("ZbT_d", [512, S], BF16)
    c.GT_d = dscr("GT_d", [24, S], BF16)
    with ExitStack() as es:
        def tsb(name, shape, dt=F32):
            return es.enter_context(nc.sbuf_tensor(name, list(shape), dt))
        wfm = tsb("wfm", [128, 8, FM0_COLS], BF16)
        wtm = tsb("wtm", [128, 8, TM0_COLS], BF16)
        hnT = tsb("hnT", [128, 8, S], BF16)
        gbc = tsb("gbc", [128, D], F32)
        xt = [tsb(f"xt{i}", [128, D], F32) for i in range(2)]
        hn = [tsb(f"hn{i}", [128, D], BF16) for i in range(2)]
        sq = tsb("sq", [128, D], BF16)
        st = [tsb(f"st{i}", [128, 4], F32) for i in range(2)]
        stage = [tsb(f"stage{i}", [128, 512], BF16) for i in range(3)]
        stage_f = tsb("stage_f", [128, 512], F32)
        b_wfm, b_wtm, b_g = Buf("wfm"), Buf("wtm"), Buf("gbc")
        b_hnT = bufs(NT, "hnT")
        b_xt, b_hn, b_st, b_sq = bufs(2, "xt"), bufs(2, "hn"), bufs(2, "st"), Buf("sq")
        b_stage, b_stage_f = bufs(3, "stage"), Buf("stagef")

        half = FM0_COLS // 2
        for kc in range(8):
            for h in range(2):
                sc.dma("pool", wfm[:, kc, h * half:(h + 1) * half],
                       c.w0_fm[kc * 128:(kc + 1) * 128, h * half:(h + 1) * half], writes=[b_wfm])
            sc.dma("pool", wtm[:, kc, :], c.w0_tm[kc * 128:(kc + 1) * 128, :], writes=[b_wtm])
        sc.dma("sp", gbc[:], c.norm_g0.partition_broadcast(128), writes=[b_g])
        sc.pool([], c.b_V, "memset", c.Vs[:, :, :, 64:65], 1.0)
        sc.pool([], c.b_V, "memset", c.Vw[:, :, :, 64:65], 1.0)
        sc.pool([], c.b_V, "memset", c.Vb[:, :, 64:65], 1.0)

        def p0(t):
            k = t % 2
            sc.dma("sp", xt[k][:], c.x[t * 128:(t + 1) * 128, :], writes=[b_xt[k]])
            sc.dve([b_xt[k]], [b_sq, b_st[k]], "scalar_tensor_tensor", out=sq[:], in0=xt[k][:], scalar=1.0 / D,
                   in1=xt[k][:], op0=ALU.mult, op1=ALU.mult, accum_out=st[k][:, 0:1])
            sc.act([b_st[k], c.b_const], [b_st[k]], "activation", out=st[k][:, 1:2], in_=st[k][:, 0:1], func=AF.Sqrt,
                   bias=c.eps_t[:, 0:1], scale=1.0)
            sc.dve([b_st[k]], [b_st[k]], "reciprocal", out=st[k][:, 2:3], in_=st[k][:, 1:2])
            sc.dve([b_xt[k], b_st[k], b_g], [b_hn[k]], "scalar_tensor_tensor", out=hn[k][:], in0=xt[k][:],
                   scalar=st[k][:, 2:3], in1=gbc[:], op0=ALU.mult, op1=ALU.mult)
            pbk = 6 + k
            tp = bank(c, pbk, 1, BF16)
            for kc in range(8):
                sc.pe([b_hn[k], c.b_const], [c.pb[pbk]], "transpose", out=tp[:, kc * 128:(kc + 1) * 128],
                      in_=hn[k][:, kc * 128:(kc + 1) * 128], identity=c.ident_b[:])
            sc.act([c.pb[pbk]], [b_hnT[t]], "activation", out=hnT[:, :, t * 128:(t + 1) * 128],
                   in_=tp.rearrange("p (c n) -> p c n", c=8), func=AF.Copy)

        nst = [0]

        def evac(kind, j, tg, ps, M, pbk):
            cols = slice(tg * 512, (tg + 1) * 512)
            if kind in ("qa", "qb", "qi"):
                scale = 0.125 if kind != "qi" else 1.0 / 16.0
                dst = {"qa": c.QaT_d, "qb": c.QbT_d, "qi": c.QiT_d}[kind]
                s = nst[0] % 3
                nst[0] += 1
                sc.dve([c.pb[pbk]], [b_stage[s]], "tensor_scalar", out=stage[s][:M, :], in0=ps, scalar1=scale,
                       scalar2=None, op0=ALU.mult)
                toks.append(sc.dma("sp", dst[j * 128:j * 128 + M, cols], stage[s][:M, :], reads=[b_stage[s]]))
            elif kind in ("za", "zb"):
                dst = {"za": c.ZaT_d, "zb": c.ZbT_d}[kind]
                s = nst[0] % 3
                nst[0] += 1
                sc.act([c.pb[pbk]], [b_stage[s]], "activation", out=stage[s][:M, :], in_=ps, func=AF.Silu)
                toks.append(sc.dma("sp", dst[j * 128:j * 128 + M, cols], stage[s][:M, :], reads=[b_stage[s]]))
            elif kind == "g":
                s = nst[0] % 3
                nst[0] += 1
                sc.act([c.pb[pbk]], [b_stage[s]], "activation", out=stage[s][:M, :], in_=ps, func=AF.Sigmoid)
                toks.append(sc.dma("sp", c.GT_d[0:M, cols], stage[s][:M, :], reads=[b_stage[s]]))
            else:
                dstt, dstb = {"kc": (c.KcT, c.b_KcT), "vc": (c.VcT, c.b_VcT), "ks": (c.KsT, c.b_KsT),
                              "kw": (c.KwT, c.b_KwT), "kbki": (c.KbKi, c.b_KbKi)}[kind]
                sc.dve([c.pb[pbk]], [dstb[tg]], "tensor_copy", out=dstt[:, cols], in_=ps)

        nb = 0
        for tg in range(NG):
            for t in range(tg * 4, tg * 4 + 4):
                p0(t)
            for t in range(tg * 4, tg * 4 + 4):
                pbk = 4 + (t % 2)
                ps = bank(c, pbk)[:, 0:TM0_COLS]
                for kc in range(8):
                    sc.pe([b_hnT[t], b_wtm], [c.pb[pbk]], "matmul", ps, lhsT=hnT[:, kc, t * 128:(t + 1) * 128],
                          rhs=wtm[:, kc, :], start=(kc == 0), stop=(kc == 7))
                sc.act([c.pb[pbk]], [c.b_V[t]], "activation", out=c.Vs[:, t, :, 0:64],
                       in_=ps[:, 0:128].rearrange("p (g d) -> p g d", g=2), func=AF.Copy)
                sc.act([c.pb[pbk]], [c.b_V[t]], "activation", out=c.Vw[:, t, :, 0:64],
                       in_=ps[:, 128:256].rearrange("p (g d) -> p g d", g=2), func=AF.Copy)
                sc.dve([c.pb[pbk]], [c.b_V[t]], "tensor_copy", out=c.Vb[:, t, 0:64], in_=ps[:, 256:320])
                sc.dve([c.pb[pbk]], [c.b_V[t]], "tensor_copy", out=c.WI[:, t, :], in_=ps[:, 320:324])
            col = 0
            for kind, ncols in FM0:
                nch = (ncols + 127) // 128
                for j in range(nch):
                    M = min(128, ncols - j * 128)
                    pbk = nb % 4
                    nb += 1
                    ps = bank(c, pbk)[:M, :]
                    for kc in range(8):
                        sc.pe([b_wfm] + b_hnT[tg * 4:tg * 4 + 4], [c.pb[pbk]], "matmul", ps,
                              lhsT=wfm[:, kc, col:col + M], rhs=hnT[:, kc, tg * 512:(tg + 1) * 512],
                              start=(kc == 0), stop=(kc == 7))
                    evac(kind, j, tg, ps, M, pbk)
                    col += M
    return toks


def _t5_bucket_np(d):
    n = np.maximum(d, 0)
    nf = np.maximum(n, 1).astype(np.float32)
    large = 16 + (np.log(nf / np.float32(16)) / np.float32(math.log(128 / 16)) * np.float32(16)).astype(np.int32)
    large = np.minimum(large, 31)
    return np.where(n < 16, n, large)


def host_consts(S, t5_table):
    NB = S // 64
    t = np.arange(S)
    cur = t // 64
    n = np.arange(NB)[None, :]
    forced = (n == 0) | (n == cur[:, None]) | (n == cur[:, None] - 1)
    F = np.where(n <= cur[:, None], 1e4 * forced, -1e30).astype(np.float32)
    tq = np.arange(128)
    d = tq[:, None] - 16 * np.arange(16)[None, :] + 113
    bc = np.where(d[:, None, :] >= 0, t5_table[_t5_bucket_np(d)][:, :, :8].transpose(0, 2, 1), NEGM)
    out = {"Fc": F, "Bc": np.ascontiguousarray(bc, np.float32)}
    for rel in (0, 1):
        d = tq[None, :] - tq[:, None] + 128 * rel
        tb = t5_table[_t5_bucket_np(d)]
        tb = np.where(d[:, :, None] >= 0, tb, NEGM).transpose(0, 2, 1)
        out[f"AB{rel}"] = np.ascontiguousarray(tb[:, :8, :], np.float32)
        out[f"DB{rel}"] = np.ascontiguousarray(tb[:, 8:, :], np.float32)
    w4 = np.where(tq[:, None] <= tq[None, :], NEGM, 0.0).astype(np.float32)
    out["W4"] = np.ascontiguousarray(np.broadcast_to(w4[:, None, :], (128, 4, 128)))
    out["tb31"] = np.ascontiguousarray(t5_table[31:32, :], np.float32)
    out["I4"] = np.ascontiguousarray(np.tile(np.eye(128, dtype=np.float32), (1, 4)))
    return out


def layer0_cmp(c):
    nc, sc, S = c.nc, c.sc, c.S
    n_cmp = S // 16 - 1
    NCP = S // 16 + 1
    NCOL = S // 16
    NCC = (n_cmp + 127) // 128
    c.n_cmp, c.NCOL, c.NCC = n_cmp, NCOL, NCC
    c.b_cmp = Buf("cmp")
    w1k_d = c.din("cmp_w1_k", [2048, 256])
    w1v_d = c.din("cmp_w1_v", [2048, 256])
    w2k_d = c.din("cmp_w2_k", [256, 64])
    w2v_d = c.din("cmp_w2_v", [256, 64])
    posk_d = c.din("cmp_posT_k", [128, 32])
    posv_d = c.din("cmp_posT_v", [128, 32])
    with ExitStack() as es:
        def tsb(name, shape, dt=F32):
            return es.enter_context(nc.sbuf_tensor(name, list(shape), dt))
        w1 = [tsb(f"w1_{i}", [128, 32, 256], BF16) for i in range(2)]
        w2kp = tsb("w2kp", [128, 2, 2, 128], BF16)
        w2v = tsb("w2v", [128, 2, 64], BF16)
        hid = tsb("hid", [128, 2, 2, 2, NCOL], BF16)
        bia = tsb("bia", [128, 8], F32)
        b_w, b_hid, b_bia = Buf("cmpw"), bufs(8, "hid"), Buf("bia")
        for i, wd in enumerate((w1k_d, w1v_d)):
            src = wd.rearrange("(l d) j -> d l j", d=64)
            for half in range(2):
                for lh in range(2):
                    sc.dma("pool", w1[i][half * 64:(half + 1) * 64, lh * 16:(lh + 1) * 16, :], src[:, lh * 16:(lh + 1) * 16, :], writes=[b_w])
        sc.pool([], [b_w], "memset", w2kp[:], 0.0)
        for g in range(2):
            sc.dma("pool", w2kp[:, :, g, g * 64:(g + 1) * 64], w2k_d.rearrange("(c p) d -> p c d", p=128), writes=[b_w])
        sc.dma("pool", w2v[:], w2v_d.rearrange("(c p) d -> p c d", p=128), writes=[b_w])
        sc.dma("pool", c.KcT[:, S:S + 32], posk_d, writes=[c.b_KcT[-1]])
        sc.dma("pool", c.VcT[:, S:S + 32], posv_d, writes=[c.b_VcT[-1]])
        n = 0
        for kv, (src, bsrc) in enumerate(((c.KcT, c.b_KcT), (c.VcT, c.b_VcT))):
            for g in range(2):
                for jc in range(2):
                    pbk = n % 4
                    ps = bank(c, pbk)[:, 0:NCP]
                    for l in range(32):
                        sc.pe(bsrc + [b_w], [c.pb[pbk]], "matmul", ps,
                              lhsT=w1[kv][g * 64:(g + 1) * 64, l, jc * 128:(jc + 1) * 128],
                              rhs=src[g * 64:(g + 1) * 64, l:l + 16 * (NCP - 1) + 1:16], start=(l == 0), stop=(l == 31))
                    sc.dve([c.pb[pbk]], [b_bia], "tensor_copy", out=bia[:, n:n + 1], in_=ps[:, NCP - 1:NCP])
                    sc.act([c.pb[pbk], b_bia], [b_hid[n]], "activation", out=hid[:, kv, g, jc, 0:n_cmp], in_=ps[:, 0:n_cmp],
                           func=AF.Silu, bias=bia[:, n:n + 1], scale=1.0)
                    n += 1
        ps = bank(c, 4)[:, 0:n_cmp]
        k = 0
        for g in range(2):
            for jc in range(2):
                sc.pe(b_hid + [b_w], [c.pb[4]], "matmul", ps, lhsT=w2kp[:, jc, g, :], rhs=hid[:, 0, g, jc, 0:n_cmp],
                      start=(k == 0), stop=(k == 3))
                k += 1
        sc.dve([c.pb[4]], [c.b_cmp], "tensor_copy", out=c.KcmpT[:, 0:n_cmp], in_=ps)
        for g in range(2):
            for cc in range(NCC):
                rows = min(128, n_cmp - cc * 128)
                pbk = 5 + (g * NCC + cc) % 2
                ps = bank(c, pbk)[0:rows, 0:64]
                for jc in range(2):
                    sc.pe(b_hid + [b_w], [c.pb[pbk]], "matmul", ps, lhsT=hid[:, 1, g, jc, cc * 128:cc * 128 + rows],
                          rhs=w2v[:, jc, :], start=(jc == 0), stop=(jc == 1))
                sc.dve([c.pb[pbk]], [c.b_cmp], "tensor_copy", out=c.Vcmp[0:rows, cc, g, :], in_=ps)
    return []


def layer0_attn(c, x1_d):
    nc, sc, S, NT = c.nc, c.sc, c.S, c.NT
    NB = S // 64
    NCOL, NCC, n_cmp = c.NCOL, c.NCC, c.n_cmp
    toks = []
    hc = c.hc
    wout_d = c.din("w_out0", [D, D])
    gw_d = c.din("ple_gw0", [D, D])
    plew_d = c.din("ple_w0", [256, D])
    p_d = c.din("p0", [S, 256])
    es = ExitStack()

    def tsb(name, shape, dt=F32):
        return es.enter_context(nc.sbuf_tensor(name, list(shape), dt))

    b_w = Buf("attw")
    wout = tsb("wout", [128, 8, D], BF16)
    Y2 = tsb("Y2", [128, 8, 128], BF16)
    b_Y2 = Buf("Y2")
    gw = tsb("gw", [128, 8, D], BF16)
    plew = tsb("plew", [128, 2, D], BF16)
    for kc in range(8):
        sc.dma("pool", wout[:, kc, :], wout_d[kc * 128:(kc + 1) * 128, :], writes=[b_w])
    for kc in range(8):
        sc.dma("pool", gw[:, kc, :], gw_d[kc * 128:(kc + 1) * 128, :], writes=[b_w])
    for kc in range(2):
        sc.dma("pool", plew[:, kc, :], plew_d[kc * 128:(kc + 1) * 128, :], writes=[b_w])
    Bc = tsb("Bc", [128, 8, 16], BF16)
    AB = [tsb(f"AB{r}", [128, 8, 128], BF16) for r in range(2)]
    DB = [tsb(f"DB{r}", [128, 8, 128], BF16) for r in range(2)]
    W4 = tsb("W4", [128, 4, 128], BF16)
    IDX = tsb("IDX", [128, S])
    stg = IDX[:, 0:1024].rearrange("p (h t) -> p h t", h=8)
    tb31 = tsb("tb31", [128, 16])
    I4 = tsb("I4", [128, 512], BF16)
    ones_f = tsb("ones_f", [128, 64])
    b_k = Buf("attc")
    sc.dma("sp", tb31[:], hc["tb31"].partition_broadcast(128), writes=[b_k])
    sc.dma("pool", W4[:], hc["W4"], writes=[b_k])
    sc.dma("sp", stg[:, :, 0:16], hc["Bc"], writes=[b_k])
    sc.dve([b_k], [b_k], "tensor_tensor", out=Bc[:], in0=stg[:, :, 0:16], in1=tb31[:, 0:8].unsqueeze(2).to_broadcast([128, 8, 16]), op=ALU.subtract)
    for r in range(2):
        sc.dma("sp", stg[:], hc[f"AB{r}"], writes=[b_k])
        sc.dve([b_k], [b_k], "tensor_tensor", out=AB[r][:], in0=stg[:], in1=tb31[:, 0:8].unsqueeze(2).to_broadcast([128, 8, 128]), op=ALU.subtract)
        sc.dma("sp", stg[:], hc[f"DB{r}"], writes=[b_k])
        sc.dve([b_k], [b_k], "tensor_tensor", out=DB[r][:], in0=stg[:], in1=tb31[:, 8:16].unsqueeze(2).to_broadcast([128, 8, 128]), op=ALU.subtract)
    sc.dma("pool", I4[:], hc["I4"], writes=[b_k])
    sc.dve([], [b_k], "memset", ones_f[:], 1.0)

    qa_t = tsb("qa_t", [128, 4, 128], BF16)
    qb_t = tsb("qb_t", [64, 8, 128], BF16)
    qi_t = tsb("qi_t", [128, 4, 128], BF16)
    za_t = tsb("za_t", [64, 8, 128], BF16)
    zb_t = tsb("zb_t", [64, 8, 128], BF16)
    g_t = tsb("g_t", [64, 24, 128], BF16)
    x_t = tsb("x_t", [128, D])
    p_t = tsb("p_t", [128, 256])
    F_t = tsb("F_t", [128, NB])
    b_q, b_z, b_g, b_x, b_p, b_F = Buf("q"), Buf("z"), Buf("g"), Buf("x"), Buf("p"), Buf("F")
    E = tsb("E", [128, 8, NCOL])
    P = E
    PS = tsb("PS", [128, 2, NCOL])
    IMP = tsb("IMP", [128, 2, NB])
    SCR = tsb("SCR", [128, 2, NB])
    T8 = tsb("T8", [128, 2, 8])
    den = tsb("den", [128, 16])
    MKs = [tsb(f"MK{g}", [128, S], BF16) for g in range(2)]
    b_MKs = bufs(2, "MK")
    PcT_flat = tsb("PcT", [128, NCC * 2 * 512], BF16)
    PcT = PcT_flat[:].rearrange("p (c g n) -> p c g n", c=NCC, g=2)
    OC = tsb("OC", [64, 8, 128])
    b_E, b_PS, b_IMP, b_SCR, b_T8, b_den, b_PcT, b_OC = (Buf(n) for n in "E PS IMP SCR T8 den PcT OC".split())
    b_P = b_E
    PT = [tsb(f"PT{i}", [128, 2, 512], BF16) for i in range(2)]
    b_PT = bufs(2, "PT")
    OS = tsb("OS", [65, 2, 512])
    OW = tsb("OW", [65, 2, 512])
    OB = tsb("OB", [65, 2, 512])
    b_OS, b_OW, b_OB = Buf("OS"), Buf("OW"), Buf("OB")
    R = tsb("R", [128, 1024])
    MD = tsb("MD", [128, S], BF16)
    bs = tsb("bs", [128, 8])
    bsi = tsb("bsi", [128, 2], I32)
    b_IDX, b_R, b_bs = Buf("IDX"), Buf("R"), Buf("bs")
    b_MD = Buf("MD")
    t1 = R[0:64, 0:512]
    t2 = R[0:64, 512:1024]
    t3 = PcT_flat[0:64, 0:1024].bitcast(F32)
    b_t1, b_t2, b_t3 = b_R, b_R, b_PcT
    YA = tsb("YA", [64, 8, 128], BF16)
    YB = tsb("YB", [64, 8, 128], BF16)
    b_Y = Buf("Y")
    XA = x_t
    XAb = tsb("XAb", [128, D], BF16)
    XAT = tsb("XAT", [128, 8, 128], BF16)
    Pb = tsb("Pb", [128, 256], BF16)
    PTT = tsb("PTT", [128, 2, 128], BF16)
    SG = tsb("SG", [128, D])
    XB = SG
    b_XAb, b_XAT, b_Pb, b_PTT, b_SG = (Buf(n) for n in "XAb XAT Pb PTT SG".split())
    b_XA, b_XB = b_x, b_SG

    sc.dve([], [b_P], "memset", P[:], 0.0)
    sc.pool([], [b_PS], "memset", PS[:], 0.0)

    pb = c.pb
    allK = c.b_KsT + c.b_KwT + c.b_KbKi
    grp = [0]

    def attn_units(i, js, kT, kbase, kb_bufs, q_rhs, b_qr, mask, b_mask, bias_fn, V_fn, b_V, acc_bank, first):
        acc = bank(c, acc_bank)[0:65, :]
        n = len(js)
        prev = None

        def pv(us, k):
            for ui, j in enumerate(us):
                sc.pe([b_PT[k], b_V[j]], [pb[acc_bank]], "matmul", acc, lhsT=V_fn(j), rhs=PT[k][:, ui, :],
                      start=(first and j == js[0]), stop=(j == js[-1]))

        for u0 in range(0, n, 2):
            us = js[u0:u0 + 2]
            k = grp[0] % 2
            grp[0] += 1
            b0 = 2 * k
            for ui, j in enumerate(us):
                ps = bank(c, b0 + ui)
                rel = i - j
                bias = bias_fn(rel)
                last_mm = "qk"
                if bias is not None:
                    last_mm = "bias"
                elif mask is not None:
                    last_mm = "mask"
                sc.pe(kb_bufs + [b_qr], [pb[b0 + ui]], "matmul", ps, lhsT=kT[kbase:kbase + 64, j * 128:(j + 1) * 128], rhs=q_rhs,
                      start=True, stop=(last_mm == "qk"))
                if mask is not None:
                    sc.pe([b_mask, b_k], [pb[b0 + ui]], "matmul", ps, lhsT=mask[:, j * 128:(j + 1) * 128], rhs=I4[:],
                          start=False, stop=(last_mm == "mask"))
                if bias is not None:
                    sc.pe([b_k, c.b_const], [pb[b0 + ui]], "matmul", ps, lhsT=c.ident_b[:], rhs=bias, start=False, stop=True)
            nu = len(us)
            sc.act([pb[b0 + ui] for ui in range(nu)], [b_PT[k]], "activation", out=PT[k][:, 0:nu, :],
                   in_=bank(c, b0, nu).rearrange("p (u n) -> p u n", u=nu), func=AF.Exp)
            if prev is not None:
                pv(*prev)
            prev = (us, k)
        if prev is not None:
            pv(*prev)

    def part_A1a(i):
        cols = slice(i * 128, (i + 1) * 128)
        N_i = (i + 1) * 128
        N_c = min(n_cmp, 8 * i + 7)
        sc.dma("sp", qa_t[:], c.QaT_d.rearrange("(m p) s -> p m s", p=128)[:, :, cols], writes=[b_q])
        sc.dma("sp", qb_t[:], c.QbT_d.rearrange("(h d) s -> d h s", d=64)[:, :, cols], writes=[b_q])
        sc.dma("sp", qi_t[64:128, :, :], c.QiT_d.rearrange("(h d) s -> d h s", d=64)[:, :, cols], writes=[b_q])
        sc.dma("sp", za_t[:], c.ZaT_d.rearrange("(h d) s -> d h s", d=64)[:, :, cols], writes=[b_z])
        sc.dma("sp", zb_t[:], c.ZbT_d.rearrange("(h d) s -> d h s", d=64)[:, :, cols], writes=[b_z])
        sc.dma("sp", g_t[:], bass.AP(tensor=c.GT_d.tensor, offset=i * 128, ap=[[0, 64], [S, 24], [1, 128]]), writes=[b_g])
        sc.dma("sp", F_t[:], hc["Fc"][cols, :], writes=[b_F])

        N_c = min(n_cmp, 8 * i + 7)
        Sc = bank(c, 0, 4).rearrange("p (h n) -> p h n", h=8)
        c_lo = max(0, 8 * i - 9)
        c_hi = min(N_c, 8 * i + 7)
        j_lo = c_lo - (8 * i - 9)
        for h in range(8):
            g, hh = divmod(h, 4)
            pbk = h // 2
            sc.pe([b_q, c.b_cmp], [pb[pbk]], "matmul", Sc[:, h, 0:N_c], lhsT=qa_t[g * 64:(g + 1) * 64, hh, :],
                  rhs=c.KcmpT[g * 64:(g + 1) * 64, 0:N_c], start=True, stop=False)
            sc.pe([b_k, c.b_const], [pb[pbk]], "matmul", Sc[:, h, c_lo:c_hi], lhsT=c.ident_b[:],
                  rhs=Bc[:, h, j_lo:j_lo + (c_hi - c_lo)], start=False, stop=True)
        for h in range(8):
            sc.act([pb[h // 2]], [b_E, b_den], "activation", out=E[:, h, 0:N_c], in_=Sc[:, h, 0:N_c], func=AF.Exp,
                   accum_out=den[:, h:h + 1])
        for k0 in range(0, N_i, 1024):
            kn = min(1024, N_i - k0)
            nbk = (kn + 511) // 512
            for hh in range(4):
                k = grp[0] % 2
                grp[0] += 1
                b0 = 2 * k
                for bq in range(nbk):
                    w = min(512, kn - bq * 512)
                    sc.pe([b_q] + c.b_KbKi, [pb[b0 + bq]], "matmul", bank(c, b0 + bq)[:, 0:w], lhsT=qi_t[64:128, hh, :],
                          rhs=c.KbKi[64:128, k0 + bq * 512:k0 + bq * 512 + w], start=True, stop=True)
                if hh == 0:
                    sc.act([pb[b0 + bq] for bq in range(nbk)], [b_R], "activation", out=R[:, 0:kn], in_=bank(c, b0, 2)[:, 0:kn], func=AF.Relu)
                    sc.dve([b_R, c.b_V[i]], [b_IDX], "tensor_scalar", out=IDX[:, k0:k0 + kn], in0=R[:, 0:kn], scalar1=c.WI[:, i, 0:1],
                           scalar2=None, op0=ALU.mult)
                else:
                    sc.act([pb[b0 + bq] for bq in range(nbk)], [b_R], "activation", out=R[:, 0:kn], in_=bank(c, b0, 2)[:, 0:kn], func=AF.Relu)
                    sc.dve([b_R, c.b_V[i], b_IDX], [b_IDX], "scalar_tensor_tensor", out=IDX[:, k0:k0 + kn], in0=R[:, 0:kn],
                           scalar=c.WI[:, i, hh:hh + 1], in1=IDX[:, k0:k0 + kn], op0=ALU.mult, op1=ALU.add)

    def part_A1b(i):
        cols = slice(i * 128, (i + 1) * 128)
        N_i = (i + 1) * 128
        N_c = min(n_cmp, 8 * i + 7)
        sc.dve([b_den], [b_den], "tensor_scalar", out=den[:, 8:16], in0=den[:, 0:8], scalar1=1e-30, scalar2=None, op0=ALU.max)
        sc.dve([b_den], [b_den], "reciprocal", out=den[:, 8:16], in_=den[:, 8:16])
        sc.dve([b_E, b_den], [b_P], "tensor_tensor", out=P[:, :, 0:N_c], in0=E[:, :, 0:N_c],
               in1=den[:, 8:16].unsqueeze(2).to_broadcast([128, 8, N_c]), op=ALU.mult)
        sc.dve([b_P], [b_PS], "tensor_reduce", out=PS[:, :, 0:N_c], in_=P[:, :, 0:N_c].rearrange("p (g h) n -> p g n h", g=2),
               axis=AX.X, op=ALU.add)
        sc.dve([b_PS], [b_IMP], "tensor_reduce", out=IMP[:], in_=PS[:].rearrange("p g (n r) -> p g n r", r=4), axis=AX.X, op=ALU.add)
        sc.dve([b_PS, b_IMP], [b_IMP], "tensor_tensor", out=IMP[:, :, 1:NB], in0=IMP[:, :, 1:NB], in1=PS[:, :, 3:4 * NB - 1:4], op=ALU.add)
        sc.dve([b_IMP, b_F], [b_SCR], "tensor_tensor", out=SCR[:], in0=IMP[:], in1=F_t[:].unsqueeze(1).to_broadcast([128, 2, NB]), op=ALU.add)
        for g in range(2):
            sc.dve([b_SCR], [b_T8], "max", out=T8[:, g, :], in_=SCR[:, g, :])
        for g in range(2):
            MK, b_MK = MKs[g], b_MKs[g]
            nblk = 2 * (i + 1)
            sc.dve([b_SCR, b_T8], [b_MK], "tensor_scalar", out=MK[:, 0:N_i].rearrange("p (n k) -> p n k", k=64),
                   in0=SCR[:, g, 0:nblk].unsqueeze(2).to_broadcast([128, nblk, 64]), scalar1=T8[:, g, 7:8], scalar2=NEGM,
                   op0=ALU.is_lt, op1=ALU.mult)
            sc.pool([b_MK], [b_MK], "affine_select", out=MK[:, cols], in_=MK[:, cols], pattern=[[-1, 128]],
                    compare_op=ALU.is_ge, fill=NEGM, base=0, channel_multiplier=1)

    def part_A1c(i):
        cols = slice(i * 128, (i + 1) * 128)
        N_i = (i + 1) * 128
        N_c = min(n_cmp, 8 * i + 7)
        sc.pool([b_IDX], [b_IDX], "affine_select", out=IDX[:, cols], in_=IDX[:, cols], pattern=[[-1, 128]],
                compare_op=ALU.is_ge, fill=-1e30, base=0, channel_multiplier=1)
        if i < 2:
            sc.dve([], [b_bs], "memset", bs[:, 2:3], -1e29)
        else:
            sub = IDX[:, 0:i * 128]
            sc.dve([b_IDX], [b_bs], "tensor_reduce", out=bs[:, 0:1], in_=sub, axis=AX.X, op=ALU.max)
            sc.dve([b_IDX], [b_bs], "tensor_reduce", out=bs[:, 2:3], in_=sub, axis=AX.X, op=ALU.min)
            sc.dve([b_bs], [b_bs], "tensor_tensor", out=bs[:, 1:2], in0=bs[:, 0:1], in1=bs[:, 2:3], op=ALU.subtract)
            for it in range(1, 21):
                sc.dve([b_bs], [b_bs], "scalar_tensor_tensor", out=bs[:, 3:4], in0=bs[:, 1:2], scalar=2.0 ** (-it), in1=bs[:, 2:3],
                       op0=ALU.mult, op1=ALU.add)
                sc.dve([b_bs, b_IDX], [b_MD, b_bs], "tensor_scalar", out=MD[:, 0:N_i], in0=IDX[:, 0:N_i], scalar1=bs[:, 3:4], scalar2=None,
                       op0=ALU.is_ge, op1=ALU.add, accum_out=bs[:, 4:5])
                sc.dve([b_bs], [b_bs], "tensor_scalar", out=bsi[:, 0:1], in0=bs[:, 4:5], scalar1=255.5, scalar2=None, op0=ALU.is_gt)
                sc.dve([b_bs], [b_bs], "copy_predicated", out=bs[:, 2:3], mask=bsi[:, 0:1], data=bs[:, 3:4])
        sc.dve([b_bs, b_IDX], [b_MD], "tensor_scalar", out=MD[:, 0:N_i], in0=IDX[:, 0:N_i], scalar1=bs[:, 2:3], scalar2=NEGM,
               op0=ALU.is_lt, op1=ALU.mult)


    def part_A2(i):
        cols = slice(i * 128, (i + 1) * 128)
        N_i = (i + 1) * 128
        N_c = min(n_cmp, 8 * i + 7)
        ncc = (N_c + 127) // 128
        for g in range(2):
            for cc in range(ncc):
                rows = min(128, N_c - cc * 128)
                pbk = 4 + (g * ncc + cc) % 2
                for hh in range(4):
                    sc.pe([b_P, c.b_const], [pb[pbk]], "transpose", out=bank(c, pbk)[0:rows, hh * 128:(hh + 1) * 128],
                          in_=P[:, g * 4 + hh, cc * 128:cc * 128 + rows], identity=c.ident_f[:])
                sc.act([pb[pbk]], [b_PcT], "activation", out=PcT[0:rows, cc, g, :], in_=bank(c, pbk)[0:rows, :], func=AF.Copy)
        for g in range(2):
            pbk = 6 + g
            for cc in range(ncc):
                rows = min(128, N_c - cc * 128)
                sc.pe([b_PcT, c.b_cmp], [pb[pbk]], "matmul", bank(c, pbk)[0:64, :], lhsT=c.Vcmp[0:rows, cc, g, :],
                      rhs=PcT[0:rows, cc, g, :], start=(cc == 0), stop=(cc == ncc - 1))
            sc.act([pb[pbk]], [b_OC], "activation", out=OC[:, g * 4:(g + 1) * 4, :],
                   in_=bank(c, pbk)[0:64, :].rearrange("p (h t) -> p h t", h=4), func=AF.Copy)


    def part_B(i):
        cols = slice(i * 128, (i + 1) * 128)
        N_i = (i + 1) * 128
        N_c = min(n_cmp, 8 * i + 7)
        for g in range(2):
            MK, b_MK = MKs[g], b_MKs[g]
            q_rhs = qa_t[g * 64:(g + 1) * 64, :, :]
            ab = lambda rel, g=g: (AB[rel][:, g * 4:(g + 1) * 4, :] if rel in (0, 1) else None)
            attn_units(i, list(range(0, i + 1)), c.KsT, g * 64, c.b_KsT, q_rhs, b_q, MK, b_MK, ab,
                       lambda j, g=g: c.Vs[:, j, g, :], c.b_V, 4 + g, True)
            sc.act([pb[4 + g]], [b_OS], "activation", out=OS[:, g, :], in_=bank(c, 4 + g)[0:65, :], func=AF.Copy)
            wb = lambda rel, g=g: (AB[rel][:, g * 4:(g + 1) * 4, :] if rel in (0, 1) else (W4[:] if rel == 4 else None))
            attn_units(i, list(range(max(0, i - 4), i + 1)), c.KwT, g * 64, c.b_KwT, q_rhs, b_q, None, None, wb,
                       lambda j, g=g: c.Vw[:, j, g, :], c.b_V, 4 + g, True)
            sc.act([pb[4 + g]], [b_OW], "activation", out=OW[:, g, :], in_=bank(c, 4 + g)[0:65, :], func=AF.Copy)

        for hg in range(2):
            db = lambda rel, hg=hg: (DB[rel][:, hg * 4:(hg + 1) * 4, :] if rel in (0, 1) else None)
            attn_units(i, list(range(0, i + 1)), c.KbKi, 0, c.b_KbKi, qb_t[:, hg * 4:(hg + 1) * 4, :], b_q, MD, b_MD, db,
                       lambda j: c.Vb[:, j, :], c.b_V, 4 + hg, True)
            sc.act([pb[4 + hg]], [b_OB], "activation", out=OB[:, hg, :], in_=bank(c, 4 + hg)[0:65, :], func=AF.Copy)

        for bi, (O, b_O) in enumerate(((OS, b_OS), (OW, b_OW), (OB, b_OB))):
            sc.act([b_O], [b_O], "activation", out=O[64:65, :, :], in_=O[64:65, :, :], func=AF.Ln)
            sc.act([b_O], [b_O], "activation", out=O[64:65, :, :], in_=O[64:65, :, :], func=AF.Exp, scale=-1.0)
        for g in range(2):
            hs = slice(g * 4, (g + 1) * 4)
            sc.pe([b_OS, b_k], [pb[6]], "matmul", bank(c, 6)[0:64, :], lhsT=ones_f[64:65, 0:64], rhs=OS[64:65, g, :], start=True, stop=True)
            sc.dve([b_OS, pb[6]], [b_t1], "tensor_tensor", out=t1[:], in0=OS[0:64, g, :], in1=bank(c, 6)[0:64, :], op=ALU.mult)
            sc.pool([b_t1, b_g], [b_t1], "tensor_tensor", out=t1[:].rearrange("p (h t) -> p h t", h=4), in0=t1[:].rearrange("p (h t) -> p h t", h=4),
                    in1=g_t[:, g * 12 + 1:g * 12 + 12:3, :], op=ALU.mult)
            sc.pe([b_OW, b_k], [pb[7]], "matmul", bank(c, 7)[0:64, :], lhsT=ones_f[64:65, 0:64], rhs=OW[64:65, g, :], start=True, stop=True)
            sc.dve([b_OW, pb[7]], [b_t2], "tensor_tensor", out=t2[:], in0=OW[0:64, g, :], in1=bank(c, 7)[0:64, :], op=ALU.mult)
            sc.pool([b_t2, b_g], [b_t2], "tensor_tensor", out=t2[:].rearrange("p (h t) -> p h t", h=4), in0=t2[:].rearrange("p (h t) -> p h t", h=4),
                    in1=g_t[:, g * 12 + 2:g * 12 + 12:3, :], op=ALU.mult)
            sc.pool([b_OC, b_g], [b_t3], "tensor_tensor", out=t3[:].rearrange("p (h t) -> p h t", h=4), in0=OC[:, hs, :],
                    in1=g_t[:, g * 12 + 0:g * 12 + 12:3, :], op=ALU.mult)
            sc.dve([b_t1, b_t2], [b_t1], "tensor_tensor", out=t1[:], in0=t1[:], in1=t2[:], op=ALU.add)
            sc.dve([b_t1, b_t3], [b_t1], "tensor_tensor", out=t1[:], in0=t1[:], in1=t3[:], op=ALU.add)
            sc.dve([b_t1, b_z], [b_Y], "tensor_tensor", out=YA[:, hs, :], in0=t1[:].rearrange("p (h t) -> p h t", h=4), in1=za_t[:, hs, :], op=ALU.mult)
            sc.pe([b_OB, b_k], [pb[6]], "matmul", bank(c, 6)[0:64, :], lhsT=ones_f[64:65, 0:64], rhs=OB[64:65, g, :], start=True, stop=True)
            sc.dve([b_OB, pb[6]], [b_t2], "tensor_tensor", out=t2[:], in0=OB[0:64, g, :], in1=bank(c, 6)[0:64, :], op=ALU.mult)
            sc.dve([b_t2, b_z], [b_Y], "tensor_tensor", out=YB[:, hs, :], in0=t2[:].rearrange("p (h t) -> p h t", h=4), in1=zb_t[:, hs, :], op=ALU.mult)


    def part_T(i):
        cols = slice(i * 128, (i + 1) * 128)
        N_i = (i + 1) * 128
        N_c = min(n_cmp, 8 * i + 7)
        sc.dma("sp", x_t[:], c.x[cols, :], writes=[b_x])
        sc.dma("sp", p_t[:], p_d[cols, :], writes=[b_p])
        for yi, Ysrc in enumerate((YA, YB)):
            for r in range(2):
                sc.dma("sp", Y2[r * 64:(r + 1) * 64, yi * 4:(yi + 1) * 4, :], Ysrc[:, r:8:2, :], reads=[b_Y], writes=[b_Y2])
        for half in range(2):
            for q in range(8):
                sc.pe([b_Y2, b_w], [pb[6 + half]], "matmul", bank(c, 6 + half), lhsT=Y2[:, q, :], rhs=wout[:, q, half * 512:(half + 1) * 512],
                      start=(q == 0), stop=(q == 7))
        sc.dve([b_x, pb[6], pb[7]], [b_XA], "tensor_tensor", out=XA[:], in0=x_t[:], in1=bank(c, 6, 2), op=ALU.add)
        sc.act([b_XA], [b_XAb], "activation", out=XAb[:], in_=XA[:], func=AF.Copy)
        sc.act([b_p], [b_Pb], "activation", out=Pb[:], in_=p_t[:], func=AF.Copy)
        tp = bank(c, 6, 1, BF16)
        for kc in range(8):
            sc.pe([b_XAb, c.b_const], [pb[6]], "transpose", out=tp[:, kc * 128:(kc + 1) * 128], in_=XAb[:, kc * 128:(kc + 1) * 128], identity=c.ident_b[:])
        sc.dve([pb[6]], [b_XAT], "tensor_copy", out=XAT[:], in_=tp.rearrange("p (c n) -> p c n", c=8))
        tp2 = bank(c, 7, 1, BF16)
        for kc in range(2):
            sc.pe([b_Pb, c.b_const], [pb[7]], "transpose", out=tp2[:, kc * 128:(kc + 1) * 128], in_=Pb[:, kc * 128:(kc + 1) * 128], identity=c.ident_b[:])
        sc.dve([pb[7]], [b_PTT], "tensor_copy", out=PTT[:], in_=tp2[:, 0:256].rearrange("p (c n) -> p c n", c=2))
        for half in range(2):
            for kc in range(8):
                sc.pe([b_XAT, b_w], [pb[4 + half]], "matmul", bank(c, 4 + half), lhsT=XAT[:, kc, :], rhs=gw[:, kc, half * 512:(half + 1) * 512],
                      start=(kc == 0), stop=(kc == 7))
        sc.act([pb[4], pb[5]], [b_SG], "activation", out=SG[:], in_=bank(c, 4, 2), func=AF.Sigmoid)
        for half in range(2):
            for kc in range(2):
                sc.pe([b_PTT, b_w], [pb[6 + half]], "matmul", bank(c, 6 + half), lhsT=PTT[:, kc, :], rhs=plew[:, kc, half * 512:(half + 1) * 512],
                      start=(kc == 0), stop=(kc == 1))
        sc.dve([b_SG, pb[6], pb[7]], [b_XB], "tensor_tensor", out=XB[:], in0=SG[:], in1=bank(c, 6, 2), op=ALU.mult)
        sc.dve([b_XB, b_XA], [b_XB], "tensor_tensor", out=XB[:], in0=XB[:], in1=XA[:], op=ALU.add)
        toks.append(sc.dma("sp", x1_d[cols, :], XB[:], reads=[b_XB]))

    for i in range(NT):
        part_A1a(i)
        if i > 0:
            part_T(i - 1)
        part_A1b(i)
        part_A2(i)
        part_A1c(i)
        part_B(i)
    part_T(NT - 1)
    es.close()
    return toks


def shared_inputs(inputs, S):
    f = lambda a: np.ascontiguousarray(np.asarray(a, dtype=np.float32))
    t5 = f(inputs["t5_table"])
    wfm, wtm = _perm_attn_w_in(f(inputs["attn_w_in"][0]))
    sh = {
        "norm_g0": f(inputs["norm_g"][0][None, :]),
        "w0_fm": wfm, "w0_tm": wtm,
        "ident": np.eye(128, dtype=np.float32),
        "cmp_w1_k": f(inputs["cmp_w1_k"][0]), "cmp_w1_v": f(inputs["cmp_w1_v"][0]),
        "cmp_w2_k": f(inputs["cmp_w2_k"][0]), "cmp_w2_v": f(inputs["cmp_w2_v"][0]),
        "cmp_posT_k": f(np.tile(np.asarray(inputs["cmp_pos_k"][0]).T, (2, 1))),
        "cmp_posT_v": f(np.tile(np.asarray(inputs["cmp_pos_v"][0]).T, (2, 1))),
        "w_out0": f(inputs["attn_w_out"][0]),
        "ple_gw0": f(inputs["ple_gate_w"][0]),
        "ple_w0": f(inputs["ple_w"][0]),
    }
    for k, v in host_consts(S, t5).items():
        sh["hc_" + k] = v
    w1fm, w1tm = _perm_rec_w_in(f(inputs["rec_w_in"][0]))
    sh["w1_fm"], sh["w1_tm"] = w1fm, w1tm
    sh["norm_g1"] = f(inputs["norm_g"][1][None, :])
    sh["final_g"] = f(np.asarray(inputs["final_g"])[None, :])
    cw = np.asarray(inputs["lru_conv_w"][0])
    vec = np.stack([cw[0], cw[1], cw[2], cw[3], np.asarray(inputs["lru_conv_b"][0]), np.asarray(inputs["lru_ba"][0]),
                    np.asarray(inputs["lru_bx"][0]), np.asarray(inputs["lru_lambda"][0])], axis=-1)
    sh["lru_vec"] = f(vec.reshape(4, 128, 8).transpose(1, 0, 2))
    for nm, key in (("lru_wa_bd", "lru_wa"), ("lru_wx_bd", "lru_wx")):
        w = np.asarray(inputs[key][0])
        bd = np.zeros((4, 128, 128), np.float32)
        for m in range(4):
            bd[m, 0:64, 0:64] = w[2 * m]
            bd[m, 64:128, 64:128] = w[2 * m + 1]
        sh[nm] = bd
    mw = np.asarray(inputs["mlstm_conv_w"][0])
    mv = np.stack([mw[0], mw[1], mw[2], mw[3], np.asarray(inputs["mlstm_conv_b"][0])], axis=-1)
    sh["ml_vec"] = f(mv.reshape(8, 128, 5).transpose(1, 0, 2))
    sh["ml_b"] = f(np.concatenate([np.asarray(inputs["mlstm_b_i"][0]), np.asarray(inputs["mlstm_b_f"][0])])[None, :])
    l1c = layer1_consts()
    sh["l1_tri"], sh["l1_e63"] = l1c["tri"], l1c["e63"]
    sh["w_out1"] = f(inputs["rec_w_out"][0])
    sh["ple_gw1"] = f(inputs["ple_gate_w"][1])
    sh["ple_w1"] = f(inputs["ple_w"][1])
    return sh


def core_inputs(inputs, b, S, shared=None):
    if shared is None:
        shared = shared_inputs(inputs, S)
    im = dict(shared)
    im["x"] = np.ascontiguousarray(np.asarray(inputs["x"][b, :S], dtype=np.float32))
    im["p0"] = np.ascontiguousarray(np.asarray(inputs["p"][0, b, :S], dtype=np.float32))
    im["p1"] = np.ascontiguousarray(np.asarray(inputs["p"][1, b, :S], dtype=np.float32))
    return im


FM1_KINDS = ["cx"] * 4 + ["cz"] * 4 + ["dq"] * 4 + ["dk"] * 4 + ["do"] * 4 + ["dz"] * 4
TM1_COLS = 520


def _perm_rec_w_in(w):
    cols_fm = np.concatenate([np.arange(0, 512), np.arange(512, 1024), np.arange(1024, 1536), np.arange(1536, 2048),
                              np.arange(2568, 3080), np.arange(3080, 3592)])
    cols_tm = np.concatenate([np.arange(2048, 2560), np.arange(2560, 2568)])
    return np.ascontiguousarray(w[:, cols_fm]), np.ascontiguousarray(w[:, cols_tm])


def layer1_consts():
    s = np.arange(64)
    tri = (s[:, None] <= s[None, :]).astype(np.float32)
    e63 = np.zeros((64, 128), np.float32)
    e63[63, :] = 1.0
    return {"tri": tri, "e63": e63}


def rms_to_hnT(c, t, k, src_rows, xt, b_xt, sq, b_sq, st, b_st, gbc, b_g, hn, b_hn, hnT, b_hnT):
    sc = c.sc
    sc.dma("sp", xt[k][:], src_rows, writes=[b_xt[k]])
    sc.dve([b_xt[k]], [b_sq, b_st[k]], "scalar_tensor_tensor", out=sq[:], in0=xt[k][:], scalar=1.0 / D,
           in1=xt[k][:], op0=ALU.mult, op1=ALU.mult, accum_out=st[k][:, 0:1])
    sc.act([b_st[k], c.b_const], [b_st[k]], "activation", out=st[k][:, 1:2], in_=st[k][:, 0:1], func=AF.Sqrt,
           bias=c.eps_t[:, 0:1], scale=1.0)
    sc.dve([b_st[k]], [b_st[k]], "reciprocal", out=st[k][:, 2:3], in_=st[k][:, 1:2])
    sc.dve([b_xt[k], b_st[k], b_g], [b_hn[k]], "scalar_tensor_tensor", out=hn[k][:], in0=xt[k][:],
           scalar=st[k][:, 2:3], in1=gbc[:], op0=ALU.mult, op1=ALU.mult)
    pbk = 6 + k
    tp = bank(c, pbk, 1, BF16)
    for kc in range(8):
        sc.pe([b_hn[k], c.b_const], [c.pb[pbk]], "transpose", out=tp[:, kc * 128:(kc + 1) * 128],
              in_=hn[k][:, kc * 128:(kc + 1) * 128], identity=c.ident_b[:])
    sc.act([c.pb[pbk]], [b_hnT[t]], "activation", out=hnT[:, :, t * 128:(t + 1) * 128],
           in_=tp.rearrange("p (c n) -> p c n", c=8), func=AF.Copy)


def layer1(c, x1_d, out_d):
    nc, sc, S, NT, NG = c.nc, c.sc, c.S, c.NT, c.NG
    NCH = S // 64
    pb = c.pb
    toks = []
    din = c.din
    w1fm_d = din("w1_fm", [D, 3072])
    w1tm_d = din("w1_tm", [D, TM1_COLS])
    g1_d = din("norm_g1", [1, D])
    gf_d = din("final_g", [1, D])
    lruv_d = din("lru_vec", [128, 4, 8])
    wabd_d = din("lru_wa_bd", [4, 128, 128])
    wxbd_d = din("lru_wx_bd", [4, 128, 128])
    mlv_d = din("ml_vec", [128, 8, 5])
    mlb_d = din("ml_b", [1, 8])
    tri_d = din("l1_tri", [64, 64])
    e63_d = din("l1_e63", [64, 128])
    wout_d = din("w_out1", [D, D])
    gw_d = din("ple_gw1", [D, D])
    plew_d = din("ple_w1", [256, D])
    p_d = din("p1", [S, 256])
    U1T_d = c.dscr("U1T_d", [3072, S], F32)

    esL = ExitStack()
    lsb = lambda name, shape, dt=F32: esL.enter_context(nc.sbuf_tensor(name, list(shape), dt))
    V1_d = c.dscr("V1_d", [S, 512], BF16)
    GIF = lsb("GIF", [64, NCH, 8])
    b_V1 = bufs(NCH, "V1")
    b_YcT, b_YdT = Buf("YcT"), bufs(NCH, "YdT")

    with ExitStack() as es:
        tsb = lambda name, shape, dt=F32: es.enter_context(nc.sbuf_tensor(name, list(shape), dt))
        wfm = tsb("w1fm", [128, 8, 3072], BF16)
        wtm = tsb("w1tm", [128, 8, TM1_COLS], BF16)
        hnT = tsb("hnT1", [128, 8, S], BF16)
        gbc = tsb("gbc1", [128, D])
        xt = [tsb(f"xt1{i}", [128, D]) for i in range(2)]
        hn = [tsb(f"hn1{i}", [128, D], BF16) for i in range(2)]
        sq = tsb("sq1", [128, D], BF16)
        st = [tsb(f"st1{i}", [128, 4]) for i in range(2)]
        stage = [tsb(f"stg1{i}", [128, 512]) for i in range(3)]
        b_wfm, b_wtm, b_g = Buf(), Buf(), Buf()
        b_hnT = bufs(NT)
        b_xt, b_hn, b_st, b_sq, b_stage = bufs(2), bufs(2), bufs(2), Buf(), bufs(3)
        for kc in range(8):
            for h in range(2):
                sc.dma("pool", wfm[:, kc, h * 1536:(h + 1) * 1536], w1fm_d[kc * 128:(kc + 1) * 128, h * 1536:(h + 1) * 1536], writes=[b_wfm])
            sc.dma("pool", wtm[:, kc, :], w1tm_d[kc * 128:(kc + 1) * 128, :], writes=[b_wtm])
        sc.dma("sp", gbc[:], g1_d.partition_broadcast(128), writes=[b_g])
        vst = [tsb(f"vst{i}", [64, 512], BF16) for i in range(2)]
        b_vst = bufs(2)
        nst = 0
        nb = 0
        for tg in range(NG):
            for t in range(tg * 4, tg * 4 + 4):
                rms_to_hnT(c, t, t % 2, x1_d[t * 128:(t + 1) * 128, :], xt, b_xt, sq, b_sq, st, b_st, gbc, b_g, hn, b_hn, hnT, b_hnT)
            for ci in range(tg * 8, tg * 8 + 8):
                t = ci // 2
                tcols = slice(ci * 64, ci * 64 + 64)
                k = ci % 2
                psv = bank(c, 4 + k)[0:64, :]
                psg = bank(c, 6 + k)[0:64, 0:8]
                for kc in range(8):
                    sc.pe([b_hnT[t], b_wtm], [pb[4 + k]], "matmul", psv, lhsT=hnT[:, kc, tcols], rhs=wtm[:, kc, 0:512], start=(kc == 0), stop=(kc == 7))
                for kc in range(8):
                    sc.pe([b_hnT[t], b_wtm], [pb[6 + k]], "matmul", psg, lhsT=hnT[:, kc, tcols], rhs=wtm[:, kc, 512:520], start=(kc == 0), stop=(kc == 7))
                sc.act([pb[4 + k]], [b_vst[k]], "activation", out=vst[k][:], in_=psv, func=AF.Copy)
                sc.dma("sp", V1_d[tcols, :], vst[k][:], reads=[b_vst[k]])
                sc.dve([pb[6 + k]], [b_V1[ci]], "tensor_copy", out=GIF[:, ci, :], in_=psg)
            for j, kind in enumerate(FM1_KINDS):
                pbk = nb % 4
                nb += 1
                ps = bank(c, pbk)
                for kc in range(8):
                    sc.pe([b_wfm] + b_hnT[tg * 4:tg * 4 + 4], [pb[pbk]], "matmul", ps, lhsT=wfm[:, kc, j * 128:(j + 1) * 128],
                          rhs=hnT[:, kc, tg * 512:(tg + 1) * 512], start=(kc == 0), stop=(kc == 7))
                s = nst % 3
                nst += 1
                if kind in ("cz", "dz"):
                    sc.act([pb[pbk]], [b_stage[s]], "activation", out=stage[s][:], in_=ps, func=AF.Silu)
                elif kind == "do":
                    sc.act([pb[pbk]], [b_stage[s]], "activation", out=stage[s][:], in_=ps, func=AF.Sigmoid)
                else:
                    sc.dve([pb[pbk]], [b_stage[s]], "tensor_copy", out=stage[s][:], in_=ps)
                sc.dma("sp", U1T_d[j * 128:(j + 1) * 128, tg * 512:(tg + 1) * 512], stage[s][:], reads=[b_stage[s]])
    sc.fence()
    YcT = lsb("YcT", [128, 4, S], BF16)
    YdT = lsb("YdT", [128, 4, S], BF16)

    with ExitStack() as es:
        tsb = lambda name, shape, dt=F32: es.enter_context(nc.sbuf_tensor(name, list(shape), dt))
        lv = tsb("lv", [128, 4, 8])
        c8 = tsb("c8", [128, 4, 4])
        wabd = tsb("wabd", [128, 4, 128], BF16)
        wxbd = tsb("wxbd", [128, 4, 128], BF16)
        b_lv = Buf()
        sc.dma("sp", lv[:], lruv_d, writes=[b_lv])
        for m in range(4):
            sc.dma("pool", wabd[:, m, :], wabd_d[m], writes=[b_lv])
            sc.dma("pool", wxbd[:, m, :], wxbd_d[m], writes=[b_lv])
        sc.act([b_lv], [b_lv], "activation", out=c8[:, :, 0], in_=lv[:, :, 7], func=AF.Exp, scale=-1.0)
        sc.act([b_lv], [b_lv], "activation", out=c8[:, :, 1], in_=c8[:, :, 0], func=AF.Ln, bias=1.0, scale=1.0)
        sc.dve([b_lv], [b_lv], "tensor_scalar", out=c8[:, :, 2], in0=c8[:, :, 1], scalar1=-8.0, scalar2=None, op0=ALU.mult)
        sc.dve([b_lv], [b_lv], "tensor_scalar", out=c8[:, :, 3], in0=c8[:, :, 1], scalar1=-16.0, scalar2=None, op0=ALU.mult)
        cx = tsb("cx", [128, S]); cz = tsb("cz", [128, S]); xc = tsb("xc", [128, S]); Rr = tsb("Rr", [128, S])
        IG = tsb("IG", [128, S]); A2 = tsb("A2", [128, S]); H = tsb("H", [128, S]); xcb = tsb("xcb", [128, S], BF16)
        b_cx, b_cz, b_xc, b_R, b_IG, b_A2, b_H, b_xcb = (Buf() for _ in range(8))
        for m in range(4):
            sc.dma("sp", cx[:], U1T_d[m * 128:(m + 1) * 128, :], writes=[b_cx])
            sc.dma("sp", cz[:], U1T_d[512 + m * 128:512 + (m + 1) * 128, :], writes=[b_cz])
            sc.dve([b_cx, b_lv], [b_xc], "tensor_scalar", out=xc[:], in0=cx[:], scalar1=lv[:, m, 3:4], scalar2=lv[:, m, 4:5], op0=ALU.mult, op1=ALU.add)
            for sh in (1, 2, 3):
                sc.dve([b_cx, b_lv, b_xc], [b_xc], "scalar_tensor_tensor", out=xc[:, sh:S], in0=cx[:, 0:S - sh], scalar=lv[:, m, 3 - sh:4 - sh],
                       in1=xc[:, sh:S], op0=ALU.mult, op1=ALU.add)
            sc.act([b_xc], [b_xcb], "activation", out=xcb[:], in_=xc[:], func=AF.Copy)
            for (wbd, dst, b_dst, bcol) in ((wabd, Rr, b_R, 5), (wxbd, IG, b_IG, 6)):
                for t0 in range(0, S, 2048):
                    tn = min(2048, S - t0)
                    nbk = tn // 512
                    for q in range(nbk):
                        sc.pe([b_xcb, b_lv], [pb[q]], "matmul", bank(c, q), lhsT=wbd[:, m, :], rhs=xcb[:, t0 + q * 512:t0 + (q + 1) * 512], start=True, stop=True)
                    sc.act([pb[q] for q in range(nbk)] + [b_lv], [b_dst], "activation", out=dst[:, t0:t0 + tn], in_=bank(c, 0, nbk), func=AF.Sigmoid,
                           bias=lv[:, m, bcol:bcol + 1], scale=1.0)
            sc.act([b_R, b_lv], [b_A2], "activation", out=A2[:], in_=Rr[:], func=AF.Exp, scale=c8[:, m, 3:4])
            sc.act([b_R, b_lv], [b_R], "activation", out=Rr[:], in_=Rr[:], func=AF.Exp, scale=c8[:, m, 2:3])
            sc.dve([b_A2], [b_A2], "tensor_scalar", out=A2[:], in0=A2[:], scalar1=-1.0, scalar2=1.0, op0=ALU.mult, op1=ALU.add)
            sc.act([b_A2], [b_A2], "activation", out=A2[:], in_=A2[:], func=AF.Sqrt)
            sc.dve([b_IG, b_xc], [b_IG], "tensor_tensor", out=IG[:], in0=IG[:], in1=xc[:], op=ALU.mult)
            sc.dve([b_IG, b_A2], [b_IG], "tensor_tensor", out=IG[:], in0=IG[:], in1=A2[:], op=ALU.mult)
            sc.dve([b_R, b_IG], [b_H], "tensor_tensor_scan", out=H[:], data0=Rr[:], data1=IG[:], initial=0.0, op0=ALU.mult, op1=ALU.add)
            sc.dve([b_H, b_cz], [b_YcT], "tensor_tensor", out=YcT[:, m, :], in0=H[:], in1=cz[:], op=ALU.mult)
    sc.fence()

    with ExitStack() as es:
        tsb = lambda name, shape, dt=F32: es.enter_context(nc.sbuf_tensor(name, list(shape), dt))
        QK = tsb("QK", [128, 8, S], BF16)
        mlv = tsb("mlv", [128, 8, 5])
        mlb = tsb("mlb", [64, 8])
        tri = tsb("tri", [64, 64])
        e63 = tsb("e63", [64, 128])
        b_ml = Buf()
        sc.dma("sp", mlv[:], mlv_d, writes=[b_ml])
        sc.dma("sp", mlb[:], mlb_d.partition_broadcast(64), writes=[b_ml])
        sc.dma("sp", tri[:], tri_d, writes=[b_ml])
        sc.dma("sp", e63[:], e63_d, writes=[b_ml])
        b_QK = Buf()
        with ExitStack() as es2:
            tsb2 = lambda name, shape, dt=F32: es2.enter_context(nc.sbuf_tensor(name, list(shape), dt))
            cu = tsb2("cu", [128, S]); xq = tsb2("xq", [128, S])
            b_cu, b_xq = Buf(), Buf()
            for j in range(8):
                sc.dma("sp", cu[:], U1T_d[1024 + j * 128:1024 + (j + 1) * 128, :], writes=[b_cu])
                sc.dve([b_cu, b_ml], [b_xq], "tensor_scalar", out=xq[:], in0=cu[:], scalar1=mlv[:, j, 3:4], scalar2=None, op0=ALU.mult)
                for sh in (1, 2, 3):
                    sc.dve([b_cu, b_ml, b_xq], [b_xq], "scalar_tensor_tensor", out=xq[:, sh:S], in0=cu[:, 0:S - sh], scalar=mlv[:, j, 3 - sh:4 - sh],
                           in1=xq[:, sh:S], op0=ALU.mult, op1=ALU.add)
                if j < 4:
                    sc.act([b_xq, b_ml], [b_QK], "activation", out=QK[:, j, :], in_=xq[:], func=AF.Silu, bias=mlv[:, j, 4:5], scale=1.0)
                else:
                    sc.act([b_xq, b_ml], [b_xq], "activation", out=xq[:], in_=xq[:], func=AF.Silu, bias=mlv[:, j, 4:5], scale=1.0)
                    sc.dve([b_xq], [b_QK], "tensor_scalar", out=QK[:, j, :], in0=xq[:], scalar1=128.0 ** -0.5, scalar2=None, op0=ALU.mult)
        sc.fence()
        LF = tsb("LF", [64, NCH, 4]); FL = tsb("FL", [64, NCH, 4]); Am = tsb("Am", [64, NCH, 4]); Bm = tsb("Bm", [64, NCH, 4])
        AE = tsb("AE", [128, NCH, 4])
        b_gt = Buf()
        sc.dve(b_V1 + [b_ml], [b_gt], "tensor_tensor", out=GIF[:], in0=GIF[:], in1=mlb[:].unsqueeze(1).to_broadcast([64, NCH, 8]), op=ALU.add)
        sc.act([b_gt], [b_gt], "activation", out=LF[:], in_=GIF[:, :, 4:8], func=AF.Exp, scale=-1.0)
        sc.act([b_gt], [b_gt], "activation", out=LF[:], in_=LF[:], func=AF.Ln, bias=1.0, scale=1.0)
        sc.dve([b_gt], [b_gt], "tensor_scalar", out=LF[:], in0=LF[:], scalar1=-1.0, scalar2=None, op0=ALU.mult)
        ng = NCH * 4
        for n0 in range(0, ng, 512):
            nn = min(512, ng - n0)
            sc.pe([b_gt, b_ml], [pb[0]], "matmul", bank(c, 0)[0:64, 0:nn], lhsT=tri[:], rhs=LF[:].rearrange("p c h -> p (c h)")[:, n0:n0 + nn], start=True, stop=True)
            sc.dve([pb[0]], [b_gt], "tensor_copy", out=FL[:].rearrange("p c h -> p (c h)")[:, n0:n0 + nn], in_=bank(c, 0)[0:64, 0:nn])
        sc.act([b_gt], [b_gt], "activation", out=Am[:], in_=FL[:], func=AF.Exp)
        sc.dve([b_gt], [b_gt], "tensor_tensor", out=Bm[:], in0=GIF[:, :, 0:4], in1=FL[:], op=ALU.subtract)
        sc.act([b_gt], [b_gt], "activation", out=Bm[:], in_=Bm[:], func=AF.Exp)
        for n0 in range(0, ng, 512):
            nn = min(512, ng - n0)
            sc.pe([b_gt, b_ml], [pb[1]], "matmul", bank(c, 1)[:, 0:nn], lhsT=e63[:], rhs=Am[:].rearrange("p c h -> p (c h)")[:, n0:n0 + nn], start=True, stop=True)
            sc.dve([pb[1]], [b_gt], "tensor_copy", out=AE[:].rearrange("p c h -> p (c h)")[:, n0:n0 + nn], in_=bank(c, 1)[:, 0:nn])

        Cst = tsb("Cst", [128, 4, 129]); Cb = tsb("Cb", [128, 4, 129], BF16)
        b_C, b_Cb = bufs(4), bufs(4)
        sc.dve([], b_C, "memset", Cst[:], 0.0)
        sc.pool([], b_Cb, "memset", Cb[:], 0.0)
        Wp = [tsb(f"Wp{i}", [64, 64], BF16) for i in range(2)]
        kB = [tsb(f"kB{i}", [64, 128], BF16) for i in range(2)]
        XY = [tsb(f"XY{i}", [64, 4, 129]) for i in range(2)]
        b_Wp, b_kB, b_XY = bufs(2), bufs(2), bufs(2)
        fin = tsb("fin", [64, 4, 4]); HD = tsb("HD", [64, 4, 128], BF16)
        Vc = [tsb(f"Vc{i}", [64, 4, 129], BF16) for i in range(2)]
        b_Vc = bufs(2)
        for i in range(2):
            sc.pool([], [b_Vc[i]], "memset", Vc[i][:, :, 128:129], 1.0)
        oz2 = [tsb(f"oz{i}", [128, 2, 4, 64]) for i in range(2)]
        b_oz2 = bufs(2)
        b_fin, b_HD = Buf(), Buf()
        sc.fence()
        Wp4 = [tsb(f"Wp4_{h}", [64, 64], BF16) for h in range(4)]
        kB4 = [tsb(f"kB4_{h}", [64, 128], BF16) for h in range(4)]
        b_Wp4, b_kB4 = bufs(4), bufs(4)
        b_ps_s, b_ps_x, b_ps_k, b_ps_u = [pb[0]] * 4, [pb[1], pb[1], pb[2], pb[2]], [pb[3]] * 4, [pb[4], pb[4], pb[5], pb[5]]
        b_XY4 = [bufs(4), bufs(4)]
        def ml_F1(ci):
            ksl = slice(ci * 64, ci * 64 + 64)
            xk = ci % 2
            oz, b_oz = oz2[xk], b_oz2[xk]
            ps_s = [bank(c, 0)[0:64, h * 64:(h + 1) * 64] for h in range(4)]
            ps_x = [bank(c, 1 + h // 2)[0:64, (h % 2) * 129:(h % 2) * 129 + 129] for h in range(4)]
            ps_k = [bank(c, 3, 1, BF16)[0:64, h * 128:(h + 1) * 128] for h in range(4)]
            ps_u = [bank(c, 4 + h // 2)[:, (h % 2) * 129:(h % 2) * 129 + 129] for h in range(4)]
            sc.dma("sp", Vc[xk][:, :, 0:128], V1_d[ksl, :].rearrange("p (h d) -> p h d", h=4), writes=[b_Vc[xk]])
            sc.dma("sp", oz[:, 0, :, :], U1T_d.rearrange("(j p) s -> p j s", p=128)[:, 16:20, ksl], writes=[b_oz])
            sc.dma("sp", oz[:, 1, :, :], U1T_d.rearrange("(j p) s -> p j s", p=128)[:, 20:24, ksl], writes=[b_oz])
            sc.pool([b_oz], [b_oz], "tensor_tensor", out=oz[:, 0, :, :], in0=oz[:, 0, :, :], in1=oz[:, 1, :, :], op=ALU.mult)
            for h in range(4):
                sc.pe([b_QK], [b_ps_s[h]], "matmul", ps_s[h], lhsT=QK[:, 4 + h, ksl], rhs=QK[:, h, ksl], start=True, stop=True)
            for h in range(4):
                sc.pe([b_QK, c.b_const], [b_ps_k[h]], "transpose", out=ps_k[h], in_=QK[:, 4 + h, ksl], identity=c.ident_b[:])
            for h in range(4):
                sc.dve([b_ps_s[h], b_gt, b_ml], [b_Wp4[h]], "scalar_tensor_tensor", out=Wp4[h][:], in0=ps_s[h], scalar=Bm[:, ci, h:h + 1], in1=tri[:],
                       op0=ALU.mult, op1=ALU.mult)
            for h in range(4):
                sc.dve([b_ps_k[h], b_gt], [b_kB4[h]], "tensor_scalar", out=kB4[h][:], in0=ps_k[h], scalar1=Bm[:, ci, h:h + 1], scalar2=AE[0:64, ci, h:h + 1],
                       op0=ALU.mult, op1=ALU.mult)

        def ml_F2(ci):
            ksl = slice(ci * 64, ci * 64 + 64)
            xk = ci % 2
            oz, b_oz = oz2[xk], b_oz2[xk]
            ps_s = [bank(c, 0)[0:64, h * 64:(h + 1) * 64] for h in range(4)]
            ps_x = [bank(c, 1 + h // 2)[0:64, (h % 2) * 129:(h % 2) * 129 + 129] for h in range(4)]
            ps_k = [bank(c, 3, 1, BF16)[0:64, h * 128:(h + 1) * 128] for h in range(4)]
            ps_u = [bank(c, 4 + h // 2)[:, (h % 2) * 129:(h % 2) * 129 + 129] for h in range(4)]
            for h in range(4):
                sc.pe([b_Wp4[h], b_Vc[xk]], [b_ps_x[h]], "matmul", ps_x[h], lhsT=Wp4[h][:], rhs=Vc[xk][:, h, :], start=True, stop=False)
                sc.pe([b_QK, b_Cb[h]], [b_ps_x[h]], "matmul", ps_x[h], lhsT=QK[:, h, ksl], rhs=Cb[:, h, :], start=False, stop=True)
            for h in range(4):
                sc.act([b_ps_x[h]], [b_XY4[xk][h]], "activation", out=XY[xk][:, h, :], in_=ps_x[h], func=AF.Copy)
            for h in range(4):
                sc.pe([b_kB4[h], b_Vc[xk]], [b_ps_u[h]], "matmul", ps_u[h], lhsT=kB4[h][:], rhs=Vc[xk][:, h, :], start=True, stop=True)

        def ml_F3(ci):
            ksl = slice(ci * 64, ci * 64 + 64)
            xk = ci % 2
            oz, b_oz = oz2[xk], b_oz2[xk]
            ps_s = [bank(c, 0)[0:64, h * 64:(h + 1) * 64] for h in range(4)]
            ps_x = [bank(c, 1 + h // 2)[0:64, (h % 2) * 129:(h % 2) * 129 + 129] for h in range(4)]
            ps_k = [bank(c, 3, 1, BF16)[0:64, h * 128:(h + 1) * 128] for h in range(4)]
            ps_u = [bank(c, 4 + h // 2)[:, (h % 2) * 129:(h % 2) * 129 + 129] for h in range(4)]
            for h in range(4):
                sc.dve([b_C[h], b_ps_u[h], b_gt], [b_C[h]], "scalar_tensor_tensor", out=Cst[:, h, :], in0=Cst[:, h, :], scalar=AE[:, ci, h:h + 1], in1=ps_u[h],
                       op0=ALU.mult, op1=ALU.add)
            for h in range(4):
                sc.act([b_C[h]], [b_Cb[h]], "activation", out=Cb[:, h, :], in_=Cst[:, h, :], func=AF.Copy)

        def ml_Ba(ci):
            ksl = slice(ci * 64, ci * 64 + 64)
            xk = ci % 2
            oz, b_oz = oz2[xk], b_oz2[xk]
            ps_s = [bank(c, 0)[0:64, h * 64:(h + 1) * 64] for h in range(4)]
            ps_x = [bank(c, 1 + h // 2)[0:64, (h % 2) * 129:(h % 2) * 129 + 129] for h in range(4)]
            ps_k = [bank(c, 3, 1, BF16)[0:64, h * 128:(h + 1) * 128] for h in range(4)]
            ps_u = [bank(c, 4 + h // 2)[:, (h % 2) * 129:(h % 2) * 129 + 129] for h in range(4)]
            X = XY[xk]
            sc.dve(b_XY4[xk] + [b_gt], [b_fin], "tensor_tensor", out=fin[:, :, 0], in0=X[:, :, 128], in1=Am[:, ci, :], op=ALU.mult)
            sc.dve([b_fin], [b_fin], "tensor_scalar", out=fin[:, :, 1], in0=fin[:, :, 0], scalar1=-1.0, scalar2=None, op0=ALU.mult)
            sc.dve([b_fin], [b_fin], "tensor_tensor", out=fin[:, :, 1], in0=fin[:, :, 1], in1=fin[:, :, 0], op=ALU.max)
            sc.dve([b_fin], [b_fin], "tensor_scalar", out=fin[:, :, 1], in0=fin[:, :, 1], scalar1=1.0, scalar2=None, op0=ALU.max)
            sc.dve([b_fin], [b_fin], "reciprocal", out=fin[:, :, 2], in_=fin[:, :, 1])
            sc.dve([b_fin, b_gt], [b_fin], "tensor_tensor", out=fin[:, :, 3], in0=fin[:, :, 2], in1=Am[:, ci, :], op=ALU.mult)
            sc.dve(b_XY4[xk] + [b_fin], [b_HD], "tensor_tensor", out=HD[:], in0=X[:, :, 0:128], in1=fin[:, :, 3:4].to_broadcast([64, 4, 128]), op=ALU.mult)

        def ml_Bb(ci):
            ksl = slice(ci * 64, ci * 64 + 64)
            xk = ci % 2
            oz, b_oz = oz2[xk], b_oz2[xk]
            ps_s = [bank(c, 0)[0:64, h * 64:(h + 1) * 64] for h in range(4)]
            ps_x = [bank(c, 1 + h // 2)[0:64, (h % 2) * 129:(h % 2) * 129 + 129] for h in range(4)]
            ps_k = [bank(c, 3, 1, BF16)[0:64, h * 128:(h + 1) * 128] for h in range(4)]
            ps_u = [bank(c, 4 + h // 2)[:, (h % 2) * 129:(h % 2) * 129 + 129] for h in range(4)]
            tpb = 6 + (ci % 2)
            tph = bank(c, tpb, 1, BF16)[:, 0:256].rearrange("p (h l) -> p h l", h=4)
            for h in range(4):
                sc.pe([b_HD, c.b_const], [pb[tpb]], "transpose", out=tph[:, h, :], in_=HD[:, h, :], identity=c.ident_b[0:64, 0:64])
            sc.dve([pb[tpb], b_oz], [b_YdT[ci]], "tensor_tensor", out=YdT[:, :, ksl], in0=tph, in1=oz[:, 0, :, :], op=ALU.mult)

        ml_F1(0)
        ml_F2(0)
        ml_F3(0)
        for ci in range(NCH):
            nxt = ci + 1 < NCH
            if nxt:
                ml_F1(ci + 1)
            ml_Ba(ci)
            if nxt:
                ml_F2(ci + 1)
            ml_Bb(ci)
            if nxt:
                ml_F3(ci + 1)
    sc.fence()

    with ExitStack() as es:
        tsb = lambda name, shape, dt=F32: es.enter_context(nc.sbuf_tensor(name, list(shape), dt))
        wout = tsb("wout1", [128, 8, D], BF16)
        gw = tsb("gw1", [128, 8, D], BF16)
        plew = tsb("plew1", [128, 2, D], BF16)
        gfb = tsb("gfb", [128, D])
        b_w = Buf()
        for kc in range(8):
            sc.dma("pool", wout[:, kc, :], wout_d[kc * 128:(kc + 1) * 128, :], writes=[b_w])
            sc.dma("pool", gw[:, kc, :], gw_d[kc * 128:(kc + 1) * 128, :], writes=[b_w])
        for kc in range(2):
            sc.dma("pool", plew[:, kc, :], plew_d[kc * 128:(kc + 1) * 128, :], writes=[b_w])
        sc.dma("sp", gfb[:], gf_d.partition_broadcast(128), writes=[b_w])
        bf = {}
        for nm, shape, dt in (("x_t", [128, D], F32), ("p_t", [128, 256], F32), ("XA", [128, D], F32), ("XAb", [128, D], BF16),
                              ("XAT", [128, 8, 128], BF16), ("Pb", [128, 256], BF16), ("PTT", [128, 2, 128], BF16), ("SG", [128, D], F32),
                              ("XB", [128, D], F32), ("sq", [128, D], BF16), ("st", [128, 4], F32), ("OUT", [128, D], F32)):
            bf[nm] = (tsb("l1" + nm, shape, dt), Buf())
        for i in range(NT):
            cols = slice(i * 128, (i + 1) * 128)
            x_t, b_x = bf["x_t"]; p_t, b_p = bf["p_t"]
            sc.dma("sp", x_t[:], x1_d[cols, :], writes=[b_x])
            sc.dma("sp", p_t[:], p_d[cols, :], writes=[b_p])
            for half in range(2):
                for kc in range(8):
                    Y = YcT if kc < 4 else YdT
                    rd = [b_YcT] if kc < 4 else [b_YdT[2 * i], b_YdT[2 * i + 1]]
                    sc.pe(rd + [b_w], [pb[6 + half]], "matmul", bank(c, 6 + half), lhsT=Y[:, kc % 4, cols], rhs=wout[:, kc, half * 512:(half + 1) * 512],
                          start=(kc == 0), stop=(kc == 7))
            toks.append(tail_tile(c, bf, gw, plew, b_w, out_d[cols, :], gfb))
    esL.close()
    sc.fence()
    return toks


def tail_tile(c, bf, gw, plew, b_w, dst, gfb):
    sc, pb = c.sc, c.pb
    x_t, b_x = bf["x_t"]; p_t, b_p = bf["p_t"]; XA, b_XA = bf["XA"]; XAb, b_XAb = bf["XAb"]; XAT, b_XAT = bf["XAT"]
    Pb, b_Pb = bf["Pb"]; PTT, b_PTT = bf["PTT"]; SG, b_SG = bf["SG"]; XB, b_XB = bf["XB"]
    sc.dve([b_x, pb[6], pb[7]], [b_XA], "tensor_tensor", out=XA[:], in0=x_t[:], in1=bank(c, 6, 2), op=ALU.add)
    sc.act([b_XA], [b_XAb], "activation", out=XAb[:], in_=XA[:], func=AF.Copy)
    sc.act([b_p], [b_Pb], "activation", out=Pb[:], in_=p_t[:], func=AF.Copy)
    tp = bank(c, 6, 1, BF16)
    for kc in range(8):
        sc.pe([b_XAb, c.b_const], [pb[6]], "transpose", out=tp[:, kc * 128:(kc + 1) * 128], in_=XAb[:, kc * 128:(kc + 1) * 128], identity=c.ident_b[:])
    sc.dve([pb[6]], [b_XAT], "tensor_copy", out=XAT[:], in_=tp.rearrange("p (c n) -> p c n", c=8))
    tp2 = bank(c, 7, 1, BF16)
    for kc in range(2):
        sc.pe([b_Pb, c.b_const], [pb[7]], "transpose", out=tp2[:, kc * 128:(kc + 1) * 128], in_=Pb[:, kc * 128:(kc + 1) * 128], identity=c.ident_b[:])
    sc.dve([pb[7]], [b_PTT], "tensor_copy", out=PTT[:], in_=tp2[:, 0:256].rearrange("p (c n) -> p c n", c=2))
    for half in range(2):
        for kc in range(8):
            sc.pe([b_XAT, b_w], [pb[4 + half]], "matmul", bank(c, 4 + half), lhsT=XAT[:, kc, :], rhs=gw[:, kc, half * 512:(half + 1) * 512],
                  start=(kc == 0), stop=(kc == 7))
    sc.act([pb[4], pb[5]], [b_SG], "activation", out=SG[:], in_=bank(c, 4, 2), func=AF.Sigmoid)
    for half in range(2):
        for kc in range(2):
            sc.pe([b_PTT, b_w], [pb[6 + half]], "matmul", bank(c, 6 + half), lhsT=PTT[:, kc, :], rhs=plew[:, kc, half * 512:(half + 1) * 512],
                  start=(kc == 0), stop=(kc == 1))
    sc.dve([b_SG, pb[6], pb[7]], [b_XB], "tensor_tensor", out=XB[:], in0=SG[:], in1=bank(c, 6, 2), op=ALU.mult)
    sc.dve([b_XB, b_XA], [b_XB], "tensor_tensor", out=XB[:], in0=XB[:], in1=XA[:], op=ALU.add)
    if gfb is None:
        return sc.dma("sp", dst, XB[:], reads=[b_XB])
    sq, b_sq = bf["sq"]; st, b_st = bf["st"]; OUT, b_OUT = bf["OUT"]
    sc.dve([b_XB], [b_sq, b_st], "scalar_tensor_tensor", out=sq[:], in0=XB[:], scalar=1.0 / D, in1=XB[:], op0=ALU.mult, op1=ALU.mult,
           accum_out=st[:, 0:1])
    sc.act([b_st, c.b_const], [b_st], "activation", out=st[:, 1:2], in_=st[:, 0:1], func=AF.Sqrt, bias=c.eps_t[:, 0:1], scale=1.0)
    sc.dve([b_st], [b_st], "reciprocal", out=st[:, 2:3], in_=st[:, 1:2])
    sc.dve([b_XB, b_st, b_w], [b_OUT], "scalar_tensor_tensor", out=OUT[:], in0=XB[:], scalar=st[:, 2:3], in1=gfb[:], op0=ALU.mult, op1=ALU.mult)
    return sc.dma("sp", dst, OUT[:], reads=[b_OUT])


_PROGRAM_CACHE = {}


def kernel(**inputs):
    S = 4096
    B = 8
    if S not in _PROGRAM_CACHE:
        _PROGRAM_CACHE[S] = build_program(S, stages=("l0", "l1"))
    nc = _PROGRAM_CACHE[S]
    shared = shared_inputs(inputs, S)
    in_maps = [core_inputs(inputs, b, S, shared) for b in range(B)]
    res = run_bass_kernel_spmd(nc, in_maps, core_ids=list(range(B)))
    out = np.stack([np.asarray(r["out"], dtype=np.float32) for r in res.results], axis=0)
    return out
```
